# Optimizing a Trainium2 kernel written in Bass

```python
import jax, jax.numpy as jnp
from jax import lax
import numpy as np

D_MODEL = 1024
BATCH = 2
SEQ = 8192
DEPTH = 2

GRID_W = 64
CTX_LEN = 256
MIX_WIDTH = D_MODEL
ML_HEADS = 4
ML_WIDTH = MIX_WIDTH // 2
ML_V_DIM = ML_WIDTH // ML_HEADS
ML_QK_DIM = ML_V_DIM // 2
HG_WIDTH = MIX_WIDTH - ML_WIDTH
HG_EXPAND = 128
HG_HEADS = HG_WIDTH // HG_EXPAND
CHUNK = 64
CONV_K = 3
N_EXPERTS = 32
TOP_K = 4
D_EXPERT = D_MODEL
SWIGLU_LIMIT = 7.0
SWIGLU_ALPHA = 1.702
MOE_BLOCK = 256
EPS = 1e-6
IN_SPLITS = (2 * ML_HEADS * ML_QK_DIM, ML_WIDTH, ML_WIDTH, 4 * ML_HEADS, 2 * HG_WIDTH, HG_WIDTH, HG_WIDTH, HG_WIDTH)
IN_WIDTH = sum(IN_SPLITS)

kernel_name = 'hybrid_mlstm_hgrn2_moe_dit'


def rms_norm(x, g):
    xf = x.astype(jnp.float32)
    y = xf * lax.rsqrt(jnp.mean(xf * xf, axis=-1, keepdims=True) + EPS)
    return (y * g).astype(x.dtype)


def adaln(cond, w, b):
    mod = (jax.nn.silu(cond) @ w + b)[..., None, :]
    return jnp.split(mod, 6, axis=-1)


def modulate(h, shift, scale):
    return h * (1 + scale) + shift


def split_cols(p):
    return jnp.split(p, np.cumsum(IN_SPLITS)[:-1].tolist(), axis=-1)


def to_heads(t, n_heads):
    b, l, _ = t.shape
    return t.reshape(b, l, n_heads, -1).transpose(0, 2, 1, 3)


def from_heads(t):
    b, h, l, d = t.shape
    return t.transpose(0, 2, 1, 3).reshape(b, l, h * d)


def head_rms(h, g):
    h = h * lax.rsqrt(jnp.mean(h * h, axis=-1, keepdims=True) + EPS)
    return from_heads(h) * g


def grid_conv(t, w, width):
    b, l, ch = t.shape
    rows = l // width
    img = t.reshape(b, rows, width, ch)
    out = lax.conv_general_dilated(img, w[:, :, None, :], window_strides=(1, 1), padding='SAME',
                                   dimension_numbers=('NHWC', 'HWIO', 'NHWC'), feature_group_count=ch)
    return jax.nn.silu(out.reshape(b, l, ch))


def to_chunks(t):
    b, h, l = t.shape[:3]
    return jnp.moveaxis(t.reshape(b, h, l // CHUNK, CHUNK, *t.shape[3:]), 2, 0)


def from_chunks(t):
    t = jnp.moveaxis(t, 0, 2)
    b, h, nc, cl = t.shape[:4]
    return t.reshape(b, h, nc * cl, *t.shape[4:])


def mlstm_scan(q, k, v, ig, lf, state):
    tri = jnp.tril(jnp.ones((CHUNK, CHUNK), bool))

    def step(carry, inp):
        C, n, m = carry
        qc, kc, vc, ic, fc = inp
        b = jnp.cumsum(fc, axis=-1)
        a_inter = b + m[..., None]
        d = jnp.where(tri, b[..., :, None] - b[..., None, :] + ic[..., None, :], -jnp.inf)
        m_t = jnp.maximum(a_inter, d.max(-1))
        s = jnp.einsum('bhtd,bhsd->bhts', qc, kc) * jnp.exp(d - m_t[..., None])
        w_inter = jnp.exp(a_inter - m_t)
        num = s @ vc + w_inter[..., None] * jnp.einsum('bhtd,bhvd->bhtv', qc, C)
        den = s.sum(-1) + w_inter * jnp.einsum('bhtd,bhd->bht', qc, n)
        h = num / jnp.maximum(jnp.abs(den), jnp.exp(-m_t))[..., None]
        g = b[..., -1]
        e = g[..., None] - b + ic
        m_new = jnp.maximum(g + m, e.max(-1))
        we = jnp.exp(e - m_new[..., None])
        wc = jnp.exp(g + m - m_new)
        C = wc[..., None, None] * C + jnp.einsum('bhs,bhsv,bhsd->bhvd', we, vc, kc)
        n = wc[..., None] * n + jnp.einsum('bhs,bhsd->bhd', we, kc)
        return (C, n, m_new), h

    state, h = lax.scan(step, state, tuple(to_chunks(t) for t in (q, k, v, ig, lf)))
    return from_chunks(h), state


def hgrn2_scan(q, k, v, lf, S):
    tri = jnp.tril(jnp.ones((CHUNK, CHUNK), bool))[:, :, None]

    def step(S, inp):
        qc, kc, vc, fc = inp
        b = jnp.cumsum(fc, axis=2)
        decay = jnp.exp(jnp.where(tri, b[:, :, :, None, :] - b[:, :, None, :, :], -jnp.inf))
        a = jnp.einsum('bhtd,bhsd,bhtsd->bhts', qc, kc, decay)
        o = a @ vc + jnp.einsum('bhtd,bhdv->bhtv', qc * jnp.exp(b), S)
        g = b[:, :, -1:]
        S = jnp.exp(g[:, :, 0])[..., None] * S + jnp.einsum('bhsd,bhsv->bhdv', kc * jnp.exp(g - b), vc)
        return S, o

    S, o = lax.scan(step, S, tuple(to_chunks(t) for t in (q, k, v, lf)))
    return from_chunks(o), S


def bidirectional(scan_fn, ctx_fwd, ctx_bwd, lat_fwd, lat_bwd, state0):
    rev = lambda ts: tuple(jnp.flip(t, axis=2) for t in ts)
    hc_f, sc_f = scan_fn(*ctx_fwd, state0)
    hl_f, _ = scan_fn(*lat_fwd, sc_f)
    hc_b, sc_b = scan_fn(*rev(ctx_bwd), state0)
    hl_b, _ = scan_fn(*rev(lat_bwd), sc_b)
    return hc_f + jnp.flip(hc_b, axis=2), hl_f + jnp.flip(hl_b, axis=2)


def mlstm_prep(qk, v, gates, conv_w, gate_b, width):
    b, l, _ = v.shape
    qk = grid_conv(qk, conv_w, width).astype(jnp.float32)
    q, k = jnp.split(qk, 2, axis=-1)
    q = to_heads(q, ML_HEADS) * ML_QK_DIM ** -0.5
    k = to_heads(k, ML_HEADS)
    v = to_heads(v.astype(jnp.float32), ML_HEADS)
    g = (gates.astype(jnp.float32).reshape(b, l, 4, ML_HEADS) + gate_b).transpose(2, 0, 3, 1)
    fwd = (q, k, v, g[0], jax.nn.log_sigmoid(g[2]))
    bwd = (q, k, v, g[1], jax.nn.log_sigmoid(g[3]))
    return fwd, bwd


def mlstm_mixer(ctx_parts, lat_parts, conv_w, gate_b, norm_g):
    c_qk, c_v, c_o, c_g = ctx_parts
    l_qk, l_v, l_o, l_g = lat_parts
    c_fwd, c_bwd = mlstm_prep(c_qk, c_v, c_g, conv_w, gate_b, c_v.shape[1])
    l_fwd, l_bwd = mlstm_prep(l_qk, l_v, l_g, conv_w, gate_b, GRID_W)
    b = c_v.shape[0]
    state0 = (jnp.zeros((b, ML_HEADS, ML_V_DIM, ML_QK_DIM), jnp.float32),
              jnp.zeros((b, ML_HEADS, ML_QK_DIM), jnp.float32),
              jnp.zeros((b, ML_HEADS), jnp.float32))
    hc, hl = bidirectional(mlstm_scan, c_fwd, c_bwd, l_fwd, l_bwd, state0)
    yc = jax.nn.sigmoid(c_o.astype(jnp.float32)) * head_rms(hc, norm_g)
    yl = jax.nn.sigmoid(l_o.astype(jnp.float32)) * head_rms(hl, norm_g)
    return yc, yl


def hgrn2_prep(qi, ff, fb, conv_w, f_b, lb, width):
    qi = grid_conv(qi, conv_w, width).astype(jnp.float32)
    q, i = jnp.split(qi, 2, axis=-1)
    q = to_heads(q, HG_HEADS)
    v = to_heads(i, HG_HEADS)

    def forget(pre, bias):
        return to_heads(lb + (1 - lb) * jax.nn.sigmoid(pre.astype(jnp.float32) + bias), HG_HEADS)

    f_fwd = forget(ff, f_b[0])
    f_bwd = forget(fb, f_b[1])
    return (q, 1 - f_fwd, v, jnp.log(f_fwd)), (q, 1 - f_bwd, v, jnp.log(f_bwd))


def hgrn2_mixer(ctx_parts, lat_parts, conv_w, f_b, lb, norm_g):
    c_qi, c_ff, c_fb, c_go = ctx_parts
    l_qi, l_ff, l_fb, l_go = lat_parts
    c_fwd, c_bwd = hgrn2_prep(c_qi, c_ff, c_fb, conv_w, f_b, lb, c_qi.shape[1])
    l_fwd, l_bwd = hgrn2_prep(l_qi, l_ff, l_fb, conv_w, f_b, lb, GRID_W)
    b = c_qi.shape[0]
    state0 = jnp.zeros((b, HG_HEADS, HG_EXPAND, HG_WIDTH // HG_HEADS), jnp.float32)
    hc, hl = bidirectional(hgrn2_scan, c_fwd, c_bwd, l_fwd, l_bwd, state0)
    yc = jax.nn.silu(c_go.astype(jnp.float32)) * head_rms(hc, norm_g)
    yl = jax.nn.silu(l_go.astype(jnp.float32)) * head_rms(hl, norm_g)
    return yc, yl


def moe_ffn(h, router_w, router_b, w_gu, b_gu, w_down, b_down):
    t, d = h.shape
    logits = (h @ router_w + router_b).astype(jnp.float32)
    top_val, top_idx = lax.top_k(logits, TOP_K)
    gates = jax.nn.softmax(top_val, axis=-1)
    n = t * TOP_K
    n_blocks = -(-n // MOE_BLOCK) + N_EXPERTS
    flat_e = top_idx.reshape(-1)
    flat_tok = jnp.repeat(jnp.arange(t, dtype=jnp.int32), TOP_K)
    order = jnp.argsort(flat_e)
    sorted_e = flat_e[order]
    counts = jnp.bincount(flat_e, length=N_EXPERTS)
    padded = (counts + MOE_BLOCK - 1) // MOE_BLOCK * MOE_BLOCK
    start = jnp.cumsum(counts) - counts
    padded_end = jnp.cumsum(padded)
    pad_start = padded_end - padded
    dest = pad_start[sorted_e] + jnp.arange(n) - start[sorted_e]
    slot_tok = jnp.full((n_blocks * MOE_BLOCK,), t, jnp.int32).at[dest].set(flat_tok[order])
    slot_gate = jnp.zeros((n_blocks * MOE_BLOCK,), jnp.float32).at[dest].set(gates.reshape(-1)[order])
    block_e = jnp.minimum(jnp.searchsorted(padded_end, jnp.arange(n_blocks) * MOE_BLOCK, side='right'), N_EXPERTS - 1)
    h_pad = jnp.concatenate([h, jnp.zeros((1, d), h.dtype)], axis=0)
    xb = h_pad[slot_tok].reshape(n_blocks, MOE_BLOCK, d)

    def expert_block(args):
        xe, e = args
        gu = xe @ w_gu[e] + b_gu[e]
        glu, lin = jnp.split(gu, 2, axis=-1)
        glu = jnp.minimum(glu, SWIGLU_LIMIT)
        lin = jnp.clip(lin, -SWIGLU_LIMIT, SWIGLU_LIMIT)
        return (glu * jax.nn.sigmoid(SWIGLU_ALPHA * glu) * (lin + 1)) @ w_down[e] + b_down[e]

    yb = lax.map(expert_block, (xb, block_e)).reshape(n_blocks * MOE_BLOCK, d)
    out = jnp.zeros_like(h_pad).at[slot_tok].add(yb * slot_gate[:, None].astype(yb.dtype))
    return out[:t]


def setup_inputs(seed: int = 0) -> dict:
    key = jax.random.key(seed)
    ks = jax.random.split(key, 28)
    nrm = lambda k, shape, scale: jax.random.normal(k, shape, jnp.float32) * scale
    D = D_MODEL
    mlstm_gate_b = jnp.concatenate([
        nrm(ks[9], (DEPTH, 2, ML_HEADS), 0.1),
        jnp.linspace(3.0, 6.0, ML_HEADS) + nrm(ks[10], (DEPTH, 2, ML_HEADS), 0.1)], axis=1)
    return {
        'x': nrm(ks[0], (BATCH, SEQ, D), 1.0),
        'c': nrm(ks[1], (BATCH, D), 1.0),
        'ctx': nrm(ks[2], (BATCH, CTX_LEN, D), 1.0),
        'c_ctx': nrm(ks[3], (D,), 1.0),
        'w_ada': nrm(ks[4], (DEPTH, D, 6 * D), 0.5 * D ** -0.5),
        'b_ada': nrm(ks[5], (DEPTH, 6 * D), 0.02),
        'norm1_g': 1.0 + nrm(ks[6], (DEPTH, D), 0.02),
        'w_in': nrm(ks[7], (DEPTH, D, IN_WIDTH), D ** -0.5),
        'mlstm_conv': nrm(ks[8], (DEPTH, CONV_K, CONV_K, 2 * ML_HEADS * ML_QK_DIM), 1.0 / CONV_K),
        'mlstm_gate_b': mlstm_gate_b,
        'mlstm_norm_g': 1.0 + nrm(ks[11], (DEPTH, ML_WIDTH), 0.02),
        'hgrn_conv': nrm(ks[12], (DEPTH, CONV_K, CONV_K, 2 * HG_WIDTH), 1.0 / CONV_K),
        'hgrn_f_b': 1.0 + nrm(ks[13], (DEPTH, 2, HG_WIDTH), 0.5),
        'hgrn_lb_raw': nrm(ks[14], (DEPTH, HG_WIDTH), 0.5),
        'hgrn_norm_g': 1.0 + nrm(ks[15], (DEPTH, HG_WIDTH), 0.02),
        'w_out': nrm(ks[16], (DEPTH, MIX_WIDTH, D), MIX_WIDTH ** -0.5),
        'norm2_g': 1.0 + nrm(ks[17], (DEPTH, D), 0.02),
        'router_w': nrm(ks[18], (DEPTH, D, N_EXPERTS), D ** -0.5),
        'router_b': nrm(ks[19], (DEPTH, N_EXPERTS), 0.01),
        'w_gu': nrm(ks[20], (DEPTH, N_EXPERTS, D, 2 * D_EXPERT), D ** -0.5),
        'b_gu': nrm(ks[21], (DEPTH, N_EXPERTS, 2 * D_EXPERT), 0.02),
        'w_down': nrm(ks[22], (DEPTH, N_EXPERTS, D_EXPERT, D), D_EXPERT ** -0.5),
        'b_down': nrm(ks[23], (DEPTH, N_EXPERTS, D), 0.02),
        'final_g': 1.0 + nrm(ks[24], (D,), 0.02),
    }


def reference(x, c, ctx, c_ctx, w_ada, b_ada, norm1_g, w_in, mlstm_conv, mlstm_gate_b, mlstm_norm_g,
              hgrn_conv, hgrn_f_b, hgrn_lb_raw, hgrn_norm_g, w_out, norm2_g, router_w, router_b,
              w_gu, b_gu, w_down, b_down, final_g):
    bsz, seq, d = x.shape
    ctx_len = ctx.shape[1]
    lb_w = jax.nn.softmax(hgrn_lb_raw.astype(jnp.float32), axis=0)
    lower_bounds = jnp.cumsum(lb_w, axis=0) - lb_w[0]
    for l in range(DEPTH):
        last = l == DEPTH - 1
        sh1, sc1, g1, sh2, sc2, g2 = adaln(c, w_ada[l], b_ada[l])
        csh1, csc1, cg1, csh2, csc2, cg2 = adaln(c_ctx, w_ada[l], b_ada[l])
        pl = split_cols(modulate(rms_norm(x, norm1_g[l]), sh1, sc1) @ w_in[l])
        pc = split_cols(modulate(rms_norm(ctx, norm1_g[l]), csh1, csc1) @ w_in[l])
        yc_ml, yl_ml = mlstm_mixer(pc[:4], pl[:4], mlstm_conv[l], mlstm_gate_b[l], mlstm_norm_g[l])
        yc_hg, yl_hg = hgrn2_mixer(pc[4:], pl[4:], hgrn_conv[l], hgrn_f_b[l], lower_bounds[l], hgrn_norm_g[l])
        x = x + g1 * (jnp.concatenate([yl_ml, yl_hg], axis=-1).astype(x.dtype) @ w_out[l])
        hl2 = modulate(rms_norm(x, norm2_g[l]), sh2, sc2).reshape(bsz * seq, d)
        if last:
            x = x + g2 * moe_ffn(hl2, router_w[l], router_b[l], w_gu[l], b_gu[l], w_down[l], b_down[l]).reshape(x.shape)
        else:
            ctx = ctx + cg1 * (jnp.concatenate([yc_ml, yc_hg], axis=-1).astype(ctx.dtype) @ w_out[l])
            hc2 = modulate(rms_norm(ctx, norm2_g[l]), csh2, csc2).reshape(bsz * ctx_len, d)
            f = moe_ffn(jnp.concatenate([hc2, hl2], axis=0), router_w[l], router_b[l], w_gu[l], b_gu[l], w_down[l], b_down[l])
            ctx = ctx + cg2 * f[:bsz * ctx_len].reshape(ctx.shape)
            x = x + g2 * f[bsz * ctx_len:].reshape(x.shape)
    return rms_norm(x, final_g)
```

```python
import numpy as np
from contextlib import ExitStack
import concourse.bass as bass
import concourse.mybir as mybir
from concourse.bass_utils import run_bass_kernel_spmd


class Buf:
    __slots__ = ("name", "w", "r")

    def __init__(self, name=""):
        self.name = name
        self.w = None
        self.r = []


class _Op:
    __slots__ = ("eng", "fn", "reads", "writes", "kind", "idx", "waits", "signal", "token",
                 "clock", "slot", "is_out")

    def __init__(self, eng, fn, reads, writes, kind):
        self.eng = eng
        self.fn = fn
        self.reads = reads
        self.writes = writes
        self.kind = kind
        self.idx = 0
        self.waits = []
        self.signal = False
        self.token = None
        self.clock = None
        self.slot = None
        self.is_out = False


ENGS = ("pe", "act", "dve", "pool", "sp")


class Sched:
    SEM_LIMIT = 30000
    NSLOT = 12

    def __init__(self, nc, es):
        self.nc = nc
        self.es = es
        self.ops = []
        self.e = {"pe": nc.tensor, "act": nc.scalar, "dve": nc.vector, "pool": nc.gpsimd, "sp": nc.sync}
        self._nsem = 0

    def _newsem(self):
        self._nsem += 1
        return self.es.enter_context(self.nc.semaphore("s%d" % self._nsem))

    def op(self, eng, fn, reads=(), writes=()):
        o = _Op(eng, fn, list(reads), list(writes), "c")
        self.ops.append(o)
        return o

    def dma(self, eng, fn, reads=(), writes=(), is_out=False, inc=16):
        o = _Op(eng, fn, list(reads), list(writes), "d")
        o.is_out = is_out
        o.idx = inc
        self.ops.append(o)
        return o

    def barrier(self):
        self.ops.append(_Op(None, None, [], [], "b"))
        self._flush(False)

    def finish(self):
        self._flush(True)
        return {"ops": self.nops, "waits": self.nwait, "sems": self._nsem}

    def _init_state(self):
        self.clock = {e: {f: 0 for f in ENGS} for e in ENGS}
        self.seen_dma = {e: set() for e in ENGS}
        self.cnt = {e: 0 for e in ENGS}
        self.last_c = {e: None for e in ENGS}
        self.slot_last = {e: [None] * self.NSLOT for e in ENGS}
        self.slot_next = {e: 0 for e in ENGS}
        self.pend_dma = []
        self.all_out = []
        self.esem = {e: self._newsem() for e in ENGS if e != "sp"}
        self.ecount = {e: 0 for e in ENGS}
        self.dsem = {e: [None] * self.NSLOT for e in ENGS}
        self.dcount = {e: [0] * self.NSLOT for e in ENGS}
        self.nwait = 0
        self.nops = 0
        self._inited = True

    def _flush(self, final):
        if not getattr(self, "_inited", False):
            self._init_state()
        clock, seen_dma, cnt, last_c = self.clock, self.seen_dma, self.cnt, self.last_c
        slot_last, slot_next = self.slot_last, self.slot_next

        def need(o, d, same_ok):
            if d is None or d is o:
                return False
            if d.kind == "d":
                return id(d) not in seen_dma[o.eng]
            if d.eng == o.eng and same_ok:
                return False
            return clock[o.eng][d.eng] < d.idx

        def add_wait(o, d):
            o.waits.append(d)
            if d.kind == "d":
                seen_dma[o.eng].add(id(d))
            else:
                d.signal = True
            ck = clock[o.eng]
            for f, v in d.clock.items():
                if v > ck[f]:
                    ck[f] = v

        ops = self.ops
        self.ops = []
        self.nops += len(ops)
        for o in ops:
            if o.kind == "b":
                lasts = [last_c[e] for e in ENGS if last_c[e] is not None]
                for e in ENGS:
                    bo = _Op(e, None, [], [], "w")
                    for d in lasts:
                        if need(bo, d, False):
                            add_wait(bo, d)
                    for d in self.pend_dma:
                        if need(bo, d, False):
                            add_wait(bo, d)
                    o.waits.append(bo)
                self.pend_dma = []
                continue
            E = o.eng
            for b in o.reads:
                d = b.w
                if d is not None and need(o, d, E == "pe"):
                    add_wait(o, d)
            for b in o.writes:
                if b.w is not None and need(o, b.w, True):
                    add_wait(o, b.w)
                for d in b.r:
                    if need(o, d, True):
                        add_wait(o, d)
            if o.kind == "d":
                sl = slot_next[E]
                slot_next[E] = (sl + 1) % self.NSLOT
                p = slot_last[E][sl]
                if p is not None and need(o, p, False):
                    add_wait(o, p)
                slot_last[E][sl] = o
                o.slot = sl
                o.clock = dict(clock[E])
                self.pend_dma.append(o)
                if o.is_out:
                    self.all_out.append(o)
            else:
                cnt[E] += 1
                o.idx = cnt[E]
                o.clock = dict(clock[E])
                o.clock[E] = o.idx
                last_c[E] = o
            for b in o.reads:
                b.r.append(o)
            for b in o.writes:
                b.w = o
                b.r = []
        fin = None
        if final:
            fin = _Op("sp", None, [], [], "w")
            for d in self.pend_dma + self.all_out:
                if need(fin, d, False):
                    add_wait(fin, d)
        def emit_waits(eng, waits):
            best = {}
            for d in waits:
                sem, val = d.token
                k = id(sem)
                if k not in best or best[k][1] < val:
                    best[k] = (sem, val)
            for sem, val in best.values():
                self.e[eng].wait_ge(sem, val)
                self.nwait += 1

        esem, ecount, dsem, dcount = self.esem, self.ecount, self.dsem, self.dcount
        for o in ops:
            if o.kind == "b":
                for bo in o.waits:
                    emit_waits(bo.eng, bo.waits)
                continue
            E = o.eng
            emit_waits(E, o.waits)
            ins = o.fn(self.e[E])
            if o.kind == "d":
                sl = o.slot
                if dsem[E][sl] is None or dcount[E][sl] + o.idx > self.SEM_LIMIT:
                    dsem[E][sl] = self._newsem()
                    dcount[E][sl] = 0
                dcount[E][sl] += o.idx
                ins.then_inc(dsem[E][sl], o.idx)
                o.token = (dsem[E][sl], dcount[E][sl])
            elif o.signal:
                if ecount[E] >= self.SEM_LIMIT:
                    esem[E] = self._newsem()
                    ecount[E] = 0
                ecount[E] += 1
                ins.then_inc(esem[E], 1)
                o.token = (esem[E], ecount[E])
            o.fn = None
        if fin is not None:
            emit_waits("sp", fin.waits)


F32 = mybir.dt.float32
BF16 = mybir.dt.bfloat16
AF = mybir.ActivationFunctionType
ALU = mybir.AluOpType
AX = mybir.AxisListType

D = 1024
NLAT = 2048
NCTX = 256
TT = 2432
HT0, HB0, CX0 = 2048, 2112, 2176
TC = 2304
NCH = 36
EPS = 1e-6
SEGS = [(0, 512, 0), (512, 1024, 0), (1024, 1536, 0), (1536, 2048, 0), (2048, 2176, 0), (2176, 2432, 1)]
C_QK, C_V, C_O, C_G, C_QI, C_FF, C_FB, C_GO = 0, 512, 1024, 1536, 1552, 2576, 3088, 3600


def chunk_col(c):
    return 64 * c if c < 32 else CX0 + 64 * (c - 32)


class Ctx:
    pass


PFX = [""]


def build(layer, last, stage, dumps, g=None, xT_src=None):
    PFX[0] = "L%d%s_" % (layer, stage[0])
    if g is None:
        nc = bass.Bass("TRN2", target_bir_lowering=False)
        es = ExitStack()
        s = Sched(nc, es)
        g = Ctx()
        g.nc, g.s, g.es = nc, s, es
        g.dump_list = []
        g.dins = {}
    nc, es, s = g.nc, g.es, g.s
    g.layer = layer

    def din(name, shape, dt=F32):
        key = "%s_L%d" % (name, layer)
        if key not in g.dins:
            g.dins[key] = nc.dram_tensor(key, list(shape), dt, kind="ExternalInput").ap()
        return g.dins[key]

    def dout(name, shape, dt=F32):
        return nc.dram_tensor(name, list(shape), dt, kind="ExternalOutput").ap()

    g.es_stage = ExitStack()

    def sb(name, shape, dt=F32):
        return g.es_stage.enter_context(nc.sbuf_tensor(PFX[0] + name, list(shape), dt))

    def ps(name, shape, dt=F32):
        return g.es_stage.enter_context(nc.psum_tensor(PFX[0] + name, list(shape), dt))

    g.din, g.dout, g.sb, g.ps = din, dout, sb, ps

    def dump(name, ap, shape, reads):
        if name not in dumps:
            return
        o = dout("dbg_" + name, shape)
        s.dma("pool", lambda e: e.dma_start(out=o, in_=ap), reads=reads, writes=[], is_out=True)
        g.dump_list.append("dbg_" + name)

    g.dump = dump
    xT = din("xT", [D, TT]) if xT_src is None else xT_src
    g.xT_d = xT
    g.dumps = dumps
    cT = din("cT", [128, 8, 2])
    consts = din("consts", [128, 384])
    w_ada = din("w_ada", [D, 6 * D])
    b_adaT = din("b_adaT", [128, 48])
    ng = din("ng", [128, 3, 8])

    cst = sb("cst", [128, 384])
    b_cst = Buf("cst")
    s.dma("sp", lambda e: e.dma_start(out=cst[:], in_=consts), writes=[b_cst])
    ident = cst[:, 0:128]
    ones = cst[:, 128:256]
    g.cst, g.b_cst, g.ident, g.ones = cst, b_cst, ident, ones
    identb = sb("identb", [128, 128], BF16)
    b_identb = Buf("identb")
    s.op("dve", lambda e: e.tensor_copy(out=identb[:], in_=ident), reads=[b_cst], writes=[b_identb])
    g.identb, g.b_identb = identb, b_identb

    ngt = sb("ngt", [128, 3, 8])
    b_ngt = Buf("ngt")
    s.dma("sp", lambda e: e.dma_start(out=ngt[:], in_=ng), writes=[b_ngt])

    mod = sb("mod", [128, 48, 2])
    b_mod = Buf("mod")
    with ExitStack() as es2:
        sc = es2.enter_context(nc.sbuf_tensor(PFX[0] + "sc", [128, 8, 2], F32))
        b_sc = Buf("sc")
        s.dma("sp", lambda e: e.dma_start(out=sc[:], in_=cT), writes=[b_sc])
        s.op("act", lambda e: e.activation(out=sc[:], in_=sc[:], func=AF.Silu), reads=[b_sc], writes=[b_sc])
        badt = es2.enter_context(nc.sbuf_tensor(PFX[0] + "badt", [128, 48], F32))
        b_badt = Buf("badt")
        s.dma("sp", lambda e: e.dma_start(out=badt[:], in_=b_adaT), writes=[b_badt])
        wa = [es2.enter_context(nc.sbuf_tensor(PFX[0] + "wa%d" % i, [128, 8, 512], F32)) for i in range(2)]
        b_wa = [Buf("wa0"), Buf("wa1")]
        pm = es2.enter_context(nc.psum_tensor(PFX[0] + "pm", [128, 48, 2], F32))
        b_pm = Buf("pm")
        w_ada_v = w_ada.rearrange("(k p) f -> p k f", p=128)
        for q in range(12):
            wt, bw = wa[q % 2], b_wa[q % 2]
            s.dma("sp", lambda e, wt=wt, q=q: e.dma_start(out=wt[:], in_=w_ada_v[:, :, q * 512:(q + 1) * 512]),
                  writes=[bw])
            for jj in range(4):
                j = q * 4 + jj
                for k in range(8):
                    s.op("pe", lambda e, wt=wt, j=j, jj=jj, k=k: e.matmul(
                        pm[:, j, :], wt[:, k, jj * 128:(jj + 1) * 128], sc[:, k, :], start=(k == 0), stop=(k == 7)),
                        reads=[bw, b_sc], writes=[b_pm])
        for c in range(2):
            s.op("dve", lambda e, c=c: e.tensor_tensor(out=mod[:, :, c], in0=pm[:, :, c], in1=badt[:],
                                                       op=ALU.add), reads=[b_pm, b_badt], writes=[b_mod])
        s.barrier()
    A1 = sb("A1", [128, 8, 2])
    A2 = sb("A2", [128, 8, 2])
    b_A = Buf("A")
    for c in range(2):
        s.op("dve", lambda e, c=c: e.scalar_tensor_tensor(out=A1[:, :, c], in0=mod[:, 8:16, c], scalar=1.0,
                                                          in1=ngt[:, 0, :], op0=ALU.add, op1=ALU.mult),
             reads=[b_mod, b_ngt], writes=[b_A])
        s.op("dve", lambda e, c=c: e.scalar_tensor_tensor(out=A2[:, :, c], in0=mod[:, 32:40, c], scalar=1.0,
                                                          in1=ngt[:, 1, :], op0=ALU.add, op1=ALU.mult),
             reads=[b_mod, b_ngt], writes=[b_A])
    g.mod, g.b_mod, g.A1, g.A2, g.b_A, g.ngt, g.b_ngt = mod, b_mod, A1, A2, b_A, ngt, b_ngt
    dump("mod", mod[:], [128, 48, 2], [b_mod])

    g.es_mix = ExitStack()
    xnT = g.es_mix.enter_context(nc.sbuf_tensor(PFX[0] + "xnT", [128, 8, TT], BF16))
    b_xn = [Buf("xn%d" % i) for i in range(len(SEGS))]
    g.xnT, g.b_xn = xnT, b_xn
    xT_v = xT.rearrange("(k p) t -> p k t", p=128)
    with ExitStack() as es2:
        xt = [es2.enter_context(nc.sbuf_tensor(PFX[0] + "xt%d" % i, [128, 8, 512], F32)) for i in range(2)]
        b_xt = [Buf("xt0"), Buf("xt1")]
        sq = es2.enter_context(nc.sbuf_tensor(PFX[0] + "sq", [128, 8, 512], F32))
        b_sq = Buf("sq")
        rs = es2.enter_context(nc.sbuf_tensor(PFX[0] + "rs", [128, 512], F32))
        b_rs = Buf("rs")
        pss = [es2.enter_context(nc.psum_tensor(PFX[0] + "pss%d" % i, [128, 512], F32)) for i in range(2)]
        b_pss = [Buf("pss0"), Buf("pss1")]
        norm_feature_major(g, xT_v, SEGS, xt, b_xt, sq, b_sq, rs, b_rs, pss, b_pss,
                           A1, lambda k, c: mod[:, 0 + k, c:c + 1], [b_A, b_mod],
                           lambda k, a, b: xnT[:, k, a:b], b_xn)
        s.barrier()
    dump("xnT", xnT[:], [128, 8, TT], b_xn)
    return g


def norm_feature_major(g, src_v, segs, xt, b_xt, sq, b_sq, rs, b_rs, pss, b_pss, A, Bcol, b_ab, outf, b_out,
                       keep_x=None):
    s, nc = g.s, g.nc
    for i, (a, b, mc) in enumerate(segs):
        w = b - a
        t, bt = xt[i % 2], b_xt[i % 2]
        pt, bp = pss[i % 2], b_pss[i % 2]
        s.dma("sp", lambda e, t=t, a=a, b=b, w=w: e.dma_start(out=t[:, :, 0:w], in_=src_v[:, :, a:b]), writes=[bt])
        s.op("act", lambda e, t=t, w=w: e.activation(out=sq[:, :, 0:w], in_=t[:, :, 0:w], func=AF.Square),
             reads=[bt], writes=[b_sq])
        for k in range(8):
            s.op("pe", lambda e, k=k, w=w, pt=pt: e.matmul(pt[:, 0:w], g.ones, sq[:, k, 0:w], start=(k == 0),
                                                           stop=(k == 7)), reads=[b_sq, g.b_cst], writes=[bp])
        s.op("act", lambda e, w=w, pt=pt: e.activation(out=rs[:, 0:w], in_=pt[:, 0:w], func=AF.Sqrt,
                                                       scale=1.0 / D, bias=EPS), reads=[bp], writes=[b_rs])
        s.op("dve", lambda e, w=w: e.reciprocal(out=rs[:, 0:w], in_=rs[:, 0:w]), reads=[b_rs], writes=[b_rs])
        for k in range(8):
            s.op("dve", lambda e, k=k, w=w, t=t, mc=mc: e.scalar_tensor_tensor(
                out=sq[:, k, 0:w], in0=t[:, k, 0:w], scalar=A[:, k, mc:mc + 1], in1=rs[:, 0:w],
                op0=ALU.mult, op1=ALU.mult), reads=[bt, b_rs] + b_ab, writes=[b_sq])
            s.op("act", lambda e, k=k, w=w, a=a, b=b, mc=mc: e.activation(
                out=outf(k, a, b), in_=sq[:, k, 0:w], func=AF.Identity, bias=Bcol(k, mc)),
                reads=[b_sq] + b_ab, writes=[b_out[i]])


def mixer_setup(g, layer):
    nc, s, din = g.nc, g.s, g.din

    def sb(name, shape, dt=F32):
        return g.es_mix.enter_context(nc.sbuf_tensor(PFX[0] + name, list(shape), dt))
    g.flags_d = din("flags", [128, 4])
    g.sel_d = din("sel", [128, 16])
    g.w_in = din("w_in", [D, 4112])
    g.w_in_v = g.w_in.rearrange("(k p) f -> p k f", p=128)
    g.cw_ml_d = din("cw_ml", [64, 8, 9])
    g.gbrep_d = din("gbrep", [64, NCH, 16])
    g.mlng_d = din("mlng", [64, 512])
    g.cw_hg_d = din("cw_hg", [128, 8, 9])
    g.fbT_d = din("fbT", [128, 2, 4])
    g.lbraw_d = din("lbraw", [128, 2, 4])
    g.hgng_d = din("hgng", [64, 512])
    sm = sb("smallc", [128, 256])
    g.b_small = Buf("small")
    g.flags = sm[:, 0:4]
    g.sel = sm[:, 4:20]
    g.fbT = sm[:, 20:28]
    g.lbraw = sm[:, 28:36]
    g.lb = sm[:, 36:40]
    g.oml = sm[:, 40:44]
    g.cw_hg = sm[:, 44:116]
    g.cw_ml = sm[0:64, 116:188]
    g.gbrep_b = sb("gbb", [64, NCH, 16])
    s.dma("sp", lambda e: e.dma_start(out=g.gbrep_b[:], in_=g.gbrep_d), writes=[g.b_small])
    for dst, src in [(g.flags, g.flags_d), (g.sel, g.sel_d), (g.fbT, g.fbT_d.rearrange("p a b -> p (a b)")),
                     (g.lbraw, g.lbraw_d.rearrange("p a b -> p (a b)")),
                     (g.cw_hg, g.cw_hg_d.rearrange("p a b -> p (a b)")),
                     (g.cw_ml, g.cw_ml_d.rearrange("p a b -> p (a b)"))]:
        s.dma("sp", lambda e, dst=dst, src=src: e.dma_start(out=dst, in_=src), writes=[g.b_small])
    if layer == 0:
        s.op("dve", lambda e: e.memset(g.lb, 0.0), writes=[g.b_small], reads=[g.b_small])
    else:
        s.op("dve", lambda e: e.tensor_tensor(out=g.lb, in0=g.lbraw[:, 4:8], in1=g.lbraw[:, 0:4], op=ALU.subtract),
             reads=[g.b_small], writes=[g.b_small])
        s.op("act", lambda e: e.activation(out=g.lb, in_=g.lb, func=AF.Sigmoid), reads=[g.b_small],
             writes=[g.b_small])
    s.op("dve", lambda e: e.tensor_scalar(out=g.oml, in0=g.lb, scalar1=-1.0, scalar2=1.0, op0=ALU.mult,
                                          op1=ALU.add), reads=[g.b_small], writes=[g.b_small])
    g.ngrep = sb("ngrep", [64, 1024])
    g.b_ngrep = Buf("ngrep")
    s.dma("sp", lambda e: e.dma_start(out=g.ngrep[:, 0:512], in_=g.mlng_d), writes=[g.b_ngrep])
    s.dma("sp", lambda e: e.dma_start(out=g.ngrep[:, 512:1024], in_=g.hgng_d), writes=[g.b_ngrep])
    g.triF = g.cst[0:64, 256:320]
    g.triB = g.cst[0:64, 320:384]
    g.ones64 = g.cst[0:64, 128:192]
    g.yT = g.es_mix.enter_context(nc.sbuf_tensor(PFX[0] + "yT", [128, 8, TC], BF16))
    g.yT_d = nc.dram_tensor(PFX[0] + "yT_d", [128, 8, TC], BF16, kind="Internal").ap()
    g.b_yTd = Buf("yTd")
    g.b_yT = [Buf("yT%d" % i) for i in range(8)]
    s.op("pool", lambda e: e.memset(g.yT[:], 0.0), writes=g.b_yT)
    nc.allow_low_precision("bf16 matmuls as in reference tolerance")


def proj_fm(g, c0, P, wt, b_wt, pj, b_pj, segs, evac):
    s = g.s
    s.dma("pool", lambda e: e.dma_start(out=wt[:, :, 0:P], in_=g.w_in_v[:, :, c0:c0 + P]), writes=[b_wt])
    for i, (a, b, mc) in enumerate(segs):
        w = b - a
        pt, bp = pj[i % 2], b_pj[i % 2]
        si = SEGS.index((a, b, mc))
        for k in range(8):
            s.op("pe", lambda e, k=k, a=a, b=b, w=w, pt=pt: e.matmul(pt[0:P, 0:w], wt[:, k, 0:P], g.xnT[:, k, a:b],
                                                                   start=(k == 0), stop=(k == 7)),
                 reads=[b_wt, g.b_xn[si]], writes=[bp])
        evac(i, a, b, pt[0:P, 0:w], bp)


def conv_silu(g, pre, b_pre, P, cw, acc, b_acc, out_bf, b_out, post_scale=None):
    s = g.s
    rd = [b_pre, g.b_small]

    def w(t):
        return cw[:, t:t + 1]
    acc3 = acc[0:P, 0:2048].rearrange("p (r c) -> p r c", c=64)
    pre3 = pre[0:P, 0:2048].rearrange("p (r c) -> p r c", c=64)
    s.op("dve", lambda e: e.tensor_scalar(out=acc[0:P, 0:2048], in0=pre[0:P, 0:2048], scalar1=w(4), scalar2=None,
                                          op0=ALU.mult), reads=rd, writes=[b_acc])
    s.op("dve", lambda e: e.tensor_scalar(out=acc[0:P, 2048:2304], in0=pre[0:P, CX0:CX0 + 256], scalar1=w(4),
                                          scalar2=None, op0=ALU.mult), reads=rd, writes=[b_acc])

    def fma(o_ap, i_ap, t):
        s.op("dve", lambda e: e.scalar_tensor_tensor(out=o_ap, in0=i_ap, scalar=w(t), in1=o_ap, op0=ALU.mult,
                                                     op1=ALU.add), reads=rd + [b_acc], writes=[b_acc])
    for dy in (-1, 0, 1):
        for dx in (-1, 0, 1):
            if dy == 0 and dx == 0:
                continue
            t = 3 * (dy + 1) + (dx + 1)
            r0, r1 = (1, 32) if dy == -1 else ((0, 31) if dy == 1 else (0, 32))
            c0, c1 = (1, 64) if dx == -1 else ((0, 63) if dx == 1 else (0, 64))
            fma(acc3[:, r0:r1, c0:c1], pre3[:, r0 + dy:r1 + dy, c0 + dx:c1 + dx], t)
            if dy == -1:
                fma(acc[0:P, c0:c1], pre[0:P, HT0 + c0 + dx:HT0 + c1 + dx], t)
            if dy == 1:
                fma(acc[0:P, 31 * 64 + c0:31 * 64 + c1], pre[0:P, HB0 + c0 + dx:HB0 + c1 + dx], t)
            if dy == 0:
                fma(acc[0:P, 2048 + c0:2048 + c0 + 255], pre[0:P, CX0 + c0 + dx:CX0 + c0 + dx + 255], t)
    s.op("act", lambda e: e.activation(out=out_bf, in_=acc[0:P, 0:TC], func=AF.Silu), reads=[b_acc], writes=[b_out])
    if post_scale is not None:
        s.op("dve", lambda e: e.tensor_scalar(out=out_bf, in0=out_bf, scalar1=post_scale, scalar2=None, op0=ALU.mult),
             reads=[b_out], writes=[b_out])


STOP = 0

ML_W = 130
HG_W = 129
NS = 8 * ML_W + 8 * HG_W


def chunk_order(d):
    if d == 0:
        return list(range(32, 36)), list(range(0, 32))
    return list(range(35, 31, -1)), list(range(31, -1, -1))


def seg_of_chunk(c):
    return min(chunk_col(c) // 512, 3) if c < 32 else 5


def mixer_phase(g, layer, stage):
    nc, s = g.nc, g.s
    full = stage == "full"
    x = Ctx()
    x.full = full
    if full:
        x.summ_all = g.summ_all_v
    else:
        x.summ_out = g.summ_d
    with ExitStack() as e2:
        def sb(name, shape, dt=F32):
            t = e2.enter_context(nc.sbuf_tensor(PFX[0] + name, list(shape), dt))
            setattr(x, name, t)
            setattr(x, "b_" + name, Buf(name))
            return t

        def ps(name, shape, dt=F32):
            t = e2.enter_context(nc.psum_tensor(PFX[0] + name, list(shape), dt))
            setattr(x, name, t)
            setattr(x, "b_" + name, Buf(name))
            return t
        x.pj = [ps("pj0", [128, 512]), ps("pj1", [128, 512])]
        x.b_pj = [x.b_pj0, x.b_pj1]
        ps("pA", [128, 64]); ps("pB", [64, 64]); ps("pO", [64, 132]); ps("pO2", [64, 132]); ps("pU", [128, 132])
        ps("pT", [128, 512], BF16)
        sb("wt", [128, 8, 256], BF16)
        sb("pre", [128, TT]); sb("acc", [128, TC])
        sb("Hacc", [64, NCH, 128])
        x.b_H = [Buf("H%d" % c) for c in range(NCH)]
        sb("gate", [64, NCH, 128], BF16)
        sb("Gall", [64, NCH, 16]); sb("LFall", [64, NCH, 8])
        sb("qT", [128, TC], BF16); sb("kT", [128, TC], BF16)
        sb("ktm", [64, NCH, 64], BF16); sb("v1", [64, NCH, 132], BF16)
        sb("BextL", [128, 2049]); sb("BextC", [128, 257]); sb("NBg", [128, 80]); sb("EG", [128, NCH])
        sb("onesr", [128, 2048], BF16)
        sb("lfc", [64, NCH]); sb("bg", [64, 2, NCH]); sb("dbias", [64, NCH]); sb("what", [64, NCH])
        sb("eg", [64, NCH]); sb("eint", [64, NCH]); sb("ssq", [64, NCH])
        sb("tmpA", [128, 4, 64]); sb("tmpB", [128, 4, 64], BF16)
        sb("trilf", [64, 64]); sb("dts", [64, 64]); sb("st", [64, 64], BF16)
        sb("stF", [64, 64], BF16); sb("stB", [64, 64], BF16)
        s.op("pool", lambda e: e.memset(x.stF[:], 0.0), writes=[x.b_stF])
        s.op("pool", lambda e: e.memset(x.stB[:], 0.0), writes=[x.b_stB])
        sb("nd", [64, 132]); sb("o2s", [64, 132]); sb("rden", [64, 2])
        sb("S32", [128, 132]); sb("Sbf", [128, 132], BF16)
        sb("kw", [64, 128], BF16)
        sb("smm", [128, 8, 130]); sb("coef", [128, 8]); sb("bsel", [128, 132])
        s.op("pool", lambda e: e.memset(x.onesr[:], 1.0), writes=[x.b_onesr])
        s.op("pool", lambda e: e.memset(x.v1[:], 1.0), writes=[x.b_v1])

        def evac_pre(P):
            def f(i, a, b, pap, bp):
                if a == HT0:
                    s.op("dve", lambda e: e.tensor_scalar(out=x.pre[0:P, HT0:HT0 + 64], in0=pap[:, 0:64],
                                                          scalar1=g.flags[0:P, 0:1], scalar2=None, op0=ALU.mult),
                         reads=[bp, g.b_small], writes=[x.b_pre])
                    s.op("dve", lambda e: e.tensor_scalar(out=x.pre[0:P, HB0:HB0 + 64], in0=pap[:, 64:128],
                                                          scalar1=g.flags[0:P, 1:2], scalar2=None, op0=ALU.mult),
                         reads=[bp, g.b_small], writes=[x.b_pre])
                elif i % 2 == 0:
                    s.op("act", lambda e: e.copy(out=x.pre[0:P, a:b], in_=pap), reads=[bp], writes=[x.b_pre])
                else:
                    s.op("dve", lambda e: e.tensor_copy(out=x.pre[0:P, a:b], in_=pap), reads=[bp], writes=[x.b_pre])
            return f
        x.evac_pre = evac_pre

        s.dma("pool", lambda e: e.dma_start(out=x.wt[:, :, 0:16], in_=g.w_in_v[:, :, C_G:C_G + 16]),
              writes=[x.b_wt])
        for grp, (c_lo, c_hi) in enumerate([(0, 32), (32, 36)]):
            pt, bp = x.pj[grp], x.b_pj[grp]
            for c in range(c_lo, c_hi):
                col = chunk_col(c)
                for k in range(8):
                    s.op("pe", lambda e, c=c, k=k, col=col, pt=pt, c_lo=c_lo: e.matmul(
                        pt[0:64, (c - c_lo) * 16:(c - c_lo + 1) * 16], g.xnT[:, k, col:col + 64], x.wt[:, k, 0:16],
                        start=(k == 0), stop=(k == 7)), reads=[x.b_wt, g.b_xn[seg_of_chunk(c)]], writes=[bp])
            n = c_hi - c_lo
            s.op("dve", lambda e, pt=pt, n=n, c_lo=c_lo, c_hi=c_hi: e.tensor_tensor(
                out=x.Gall[:, c_lo:c_hi, :], in0=pt[0:64, 0:n * 16].rearrange("p (c x) -> p c x", x=16),
                in1=g.gbrep_b[:, c_lo:c_hi, :], op=ALU.add), reads=[bp, g.b_small], writes=[x.b_Gall])
        s.op("act", lambda e: e.activation(out=x.LFall[:], in_=x.Gall[:, :, 8:16], func=AF.Exp, scale=-1.0),
             reads=[x.b_Gall], writes=[x.b_LFall])
        s.op("act", lambda e: e.activation(out=x.LFall[:], in_=x.LFall[:], func=AF.Ln, bias=1.0),
             reads=[x.b_LFall], writes=[x.b_LFall])
        s.op("dve", lambda e: e.tensor_scalar(out=x.LFall[:], in0=x.LFall[:], scalar1=-1.0, scalar2=None,
                                              op0=ALU.mult), reads=[x.b_LFall], writes=[x.b_LFall])
        g.dump("Gall", x.Gall[:], [64, NCH, 16], [x.b_Gall])
        g.dump("LFall", x.LFall[:], [64, NCH, 8], [x.b_LFall])
        for h in g.ml_heads:
            ml_head(g, x, layer, h)
        for h in g.hg_heads:
            hg_head(g, x, layer, h)
        if full:
            s.dma("sp", lambda e: e.dma_start(out=g.yT_d, in_=g.yT[:]), reads=g.b_yT, writes=[g.b_yTd])
        s.barrier()
    g.es_mix.close()


def fold_state(g, x, P, W, base):
    s = g.s
    if STOP == 2:
        return
    d = x.cur_dir
    s.dma("sp", lambda e: e.dma_start(out=x.smm[0:P, :, 0:W + 1],
                                       in_=x.summ_all[:, 0:P, base:base + W + 1].rearrange("j p c -> p j c")),
          reads=[g.b_gath], writes=[x.b_smm])
    sel = g.sel[0:P, d * 8:d * 8 + 8]
    s.op("dve", lambda e: e.scalar_tensor_tensor(out=x.coef[0:P, :], in0=x.smm[0:P, :, W], scalar=-1.0, in1=sel,
                                                 op0=ALU.add, op1=ALU.mult), reads=[x.b_smm, g.b_small],
         writes=[x.b_coef])
    s.op("dve", lambda e: e.tensor_scalar(out=x.coef[0:P, :], in0=x.coef[0:P, :], scalar1=1.0, scalar2=None,
                                          op0=ALU.add), reads=[x.b_coef], writes=[x.b_coef])
    order = range(8) if d == 0 else range(7, -1, -1)
    for j in order:
        s.op("dve", lambda e, j=j: e.tensor_scalar(out=x.bsel[0:P, 0:W], in0=x.smm[0:P, j, 0:W],
                                                   scalar1=g.sel[0:P, d * 8 + j:d * 8 + j + 1], scalar2=None,
                                                   op0=ALU.mult), reads=[x.b_smm, g.b_small], writes=[x.b_bsel])
        s.op("dve", lambda e, j=j: e.scalar_tensor_tensor(out=x.S32[0:P, 0:W], in0=x.S32[0:P, 0:W],
                                                          scalar=x.coef[0:P, j:j + 1], in1=x.bsel[0:P, 0:W],
                                                          op0=ALU.mult, op1=ALU.add),
             reads=[x.b_S32, x.b_coef, x.b_bsel], writes=[x.b_S32])
    s.op("act", lambda e: e.copy(out=x.Sbf[0:P, 0:W], in_=x.S32[0:P, 0:W]), reads=[x.b_S32], writes=[x.b_Sbf])


def write_summary(g, x, P, W, base):
    s = g.s
    s.dma("sp", lambda e: e.dma_start(out=x.summ_out[0:P, base:base + W + 1], in_=x.S32[0:P, 0:W + 1]),
          reads=[x.b_S32])


def out_stage(g, x, ng_ap, kchunk):
    s = g.s
    ss = x.ssq
    for half in range(2):
        c0, c1 = half * 18, half * 18 + 18
        sq3 = x.acc[0:64, 0:18 * 128].rearrange("p (c v) -> p c v", v=128)
        s.op("dve", lambda e, c0=c0, c1=c1, sq3=sq3: e.tensor_tensor(
            out=sq3, in0=x.Hacc[:, c0:c1, :], in1=x.Hacc[:, c0:c1, :], op=ALU.mult),
            reads=x.b_H[c0:c1], writes=[x.b_acc])
        s.op("dve", lambda e, c0=c0, c1=c1, sq3=sq3: e.tensor_reduce(out=ss[:, c0:c1], in_=sq3, axis=AX.X,
                                                                      op=ALU.add), reads=[x.b_acc], writes=[x.b_ssq])
    s.op("act", lambda e: e.activation(out=ss[:], in_=ss[:], func=AF.Sqrt, scale=1.0 / 128, bias=EPS),
         reads=[x.b_ssq], writes=[x.b_ssq])
    s.op("dve", lambda e: e.reciprocal(out=ss[:], in_=ss[:]), reads=[x.b_ssq], writes=[x.b_ssq])
    for c in range(NCH):
        s.op("dve", lambda e, c=c: e.scalar_tensor_tensor(out=x.Hacc[:, c, :], in0=x.Hacc[:, c, :],
                                                          scalar=ss[:, c:c + 1], in1=ng_ap, op0=ALU.mult,
                                                          op1=ALU.mult),
             reads=[x.b_H[c], x.b_ssq, g.b_ngrep], writes=[x.b_H[c]])
    s.op("dve", lambda e: e.tensor_tensor(out=x.gate[:], in0=x.Hacc[:], in1=x.gate[:], op=ALU.mult),
         reads=x.b_H + [x.b_gate], writes=[x.b_gate])
    for q in range(NCH // 4):
        for j in range(4):
            c = q * 4 + j
            s.op("pe", lambda e, c=c, j=j: e.transpose(x.pT[:, j * 64:(j + 1) * 64], x.gate[:, c, :],
                                                       g.identb[0:64, 0:64]),
                 reads=[x.b_gate, g.b_identb], writes=[x.b_pT])
        s.op("act", lambda e, q=q: e.copy(out=g.yT[:, kchunk, q * 256:(q + 1) * 256], in_=x.pT[:, 0:256]),
             reads=[x.b_pT], writes=[g.b_yT[kchunk]])


def proj_tm(g, x, c0, N, per, evac):
    s = g.s
    s.dma("pool", lambda e: e.dma_start(out=x.wt[:, :, 0:N], in_=g.w_in_v[:, :, c0:c0 + N]), writes=[x.b_wt])
    for q in range(NCH // per):
        pt, bp = x.pj[q % 2], x.b_pj[q % 2]
        for j in range(per):
            c = q * per + j
            col = chunk_col(c)
            for k in range(8):
                s.op("pe", lambda e, k=k, col=col, j=j, pt=pt: e.matmul(
                    pt[0:64, j * N:(j + 1) * N], g.xnT[:, k, col:col + 64], x.wt[:, k, 0:N], start=(k == 0),
                    stop=(k == 7)), reads=[x.b_wt, g.b_xn[seg_of_chunk(c)]], writes=[bp])
        evac(q * per, per, pt[0:64, 0:per * N].rearrange("p (c n) -> p c n", n=N), bp)


def ml_head(g, x, layer, h):
    s = g.s
    full = x.full
    for which, (c0, dst, bdst, scl) in enumerate([(C_QK + 64 * h, x.qT, x.b_qT, 0.125),
                                                  (C_QK + 256 + 64 * h, x.kT, x.b_kT, None)]):
        proj_fm(g, c0, 64, x.wt, x.b_wt, x.pj, x.b_pj, SEGS, x.evac_pre(64))
        cw = g.cw_ml[:, (which * 4 + h) * 9:(which * 4 + h) * 9 + 9]
        conv_silu(g, x.pre, x.b_pre, 64, cw, x.acc, x.b_acc, dst[0:64, :], bdst, post_scale=scl)
    for q in range(NCH // 6):
        for j in range(6):
            c = q * 6 + j
            s.op("pe", lambda e, c=c, j=j: e.transpose(x.pT[0:64, j * 64:(j + 1) * 64], x.kT[0:64, c * 64:c * 64 + 64],
                                                       g.identb[0:64, 0:64]), reads=[x.b_kT, g.b_identb],
                 writes=[x.b_pT])
        s.op("act", lambda e, q=q: e.copy(out=x.ktm[:, q * 6:(q + 1) * 6, :],
                                          in_=x.pT[0:64, 0:384].rearrange("p (c d) -> p c d", d=64)),
             reads=[x.b_pT], writes=[x.b_ktm])
    def evac_v(c_lo, n, p3, bp):
        s.op("dve", lambda e: e.tensor_copy(out=x.v1[:, c_lo:c_lo + n, 0:128], in_=p3[:, :, 0:128]), reads=[bp],
             writes=[x.b_v1])
    proj_tm_pair(g, x, [(C_V + 128 * h, 128)], 4, evac_v)
    if full:
        def evac_o(c_lo, n, p3, bp):
            s.op("act", lambda e: e.activation(out=x.gate[:, c_lo:c_lo + n, :], in_=p3, func=AF.Sigmoid),
                 reads=[bp], writes=[x.b_gate])
        proj_tm_pair(g, x, [(C_O + 128 * h, 128)], 4, evac_o)
    for d in range(2):
        ml_dir(g, x, layer, h, d)
    if full:
        g.dump("ml_qT", x.qT[0:64, :], [64, TC], [x.b_qT])
        g.dump("ml_kT", x.kT[0:64, :], [64, TC], [x.b_kT])
        g.dump("ml_v1", x.v1[:], [64, NCH, 132], [x.b_v1])
        g.dump("ml_gate", x.gate[:], [64, NCH, 128], [x.b_gate])
        g.dump("ml_H", x.Hacc[:], [64, NCH, 128], x.b_H)
        g.dump("ml_ktm", x.ktm[:], [64, NCH, 64], [x.b_ktm])
    if full and STOP != 3:
        out_stage(g, x, g.ngrep[:, 128 * h:128 * h + 128], h)


def ml_dir(g, x, layer, h, d):
    s = g.s
    full = x.full
    x.cur_dir = d
    tri = g.triF if d == 0 else g.triB
    s.op("dve", lambda e: e.tensor_copy(out=x.lfc[:], in_=x.LFall[:, :, d * 4 + h]), reads=[x.b_LFall],
         writes=[x.b_lfc])
    s.op("pe", lambda e: e.matmul(x.pO2[:, 0:NCH], tri, x.lfc[:], start=True, stop=True),
         reads=[x.b_lfc, g.b_cst], writes=[x.b_pO2])
    s.op("pe", lambda e: e.matmul(x.pO2[:, 64:64 + NCH], g.ones64, x.lfc[:], start=True, stop=True),
         reads=[x.b_lfc, g.b_cst], writes=[x.b_pO2])
    s.op("dve", lambda e: e.tensor_copy(out=x.bg[:, 0, :], in_=x.pO2[:, 0:NCH]), reads=[x.b_pO2], writes=[x.b_bg])
    s.op("dve", lambda e: e.tensor_copy(out=x.bg[:, 1, :], in_=x.pO2[:, 64:64 + NCH]), reads=[x.b_pO2],
         writes=[x.b_bg])
    bcol, gall = x.bg[:, 0, :], x.bg[:, 1, :]
    s.op("dve", lambda e: e.tensor_tensor(out=x.dbias[:], in0=x.Gall[:, :, d * 4 + h], in1=bcol, op=ALU.subtract),
         reads=[x.b_Gall, x.b_bg], writes=[x.b_dbias])
    s.op("dve", lambda e: e.tensor_tensor(out=x.what[:], in0=x.dbias[:], in1=gall, op=ALU.add),
         reads=[x.b_dbias, x.b_bg], writes=[x.b_what])
    s.op("act", lambda e: e.activation(out=x.what[:], in_=x.what[:], func=AF.Exp), reads=[x.b_what],
         writes=[x.b_what])
    s.op("act", lambda e: e.activation(out=x.eg[:], in_=gall, func=AF.Exp), reads=[x.b_bg], writes=[x.b_eg])
    s.op("act", lambda e: e.activation(out=x.eint[:], in_=bcol, func=AF.Exp), reads=[x.b_bg], writes=[x.b_eint])
    ctxc, latc = chunk_order(d)
    base = (h * 2 + d) * ML_W

    def zero_state():
        s.op("dve", lambda e: e.memset(x.S32[:], 0.0), writes=[x.b_S32])
        s.op("pool", lambda e: e.memset(x.Sbf[:], 0.0), writes=[x.b_Sbf])
    zero_state()
    if not full:
        for c in latc:
            ml_chunk(g, x, h, d, c, tri, state_only=True, first=False)
        s.op("dve", lambda e: e.tensor_reduce(out=x.rden[:, 0:1], in_=x.bg[:, 1, 0:32], axis=AX.X, op=ALU.add),
             reads=[x.b_bg], writes=[x.b_rden])
        s.op("act", lambda e: e.activation(out=x.S32[0:64, 129:130], in_=x.rden[:, 0:1], func=AF.Exp),
             reads=[x.b_rden], writes=[x.b_S32])
        write_summary(g, x, 64, 129, base)
        return
    for i, c in enumerate(ctxc):
        ml_chunk(g, x, h, d, c, tri, state_only=False, first=(i == 0))
    fold_state(g, x, 64, 129, base)
    for c in latc:
        ml_chunk(g, x, h, d, c, tri, state_only=False, first=False)


def proj_tm_pair(g, x, cols, per, evac):
    s = g.s
    N = 128 * len(cols)
    for i, (c0, n) in enumerate(cols):
        s.dma("pool", lambda e, i=i, c0=c0, n=n: e.dma_start(out=x.wt[:, :, i * 128:i * 128 + n],
                                                             in_=g.w_in_v[:, :, c0:c0 + n]), writes=[x.b_wt])
    for q in range(NCH // per):
        pt, bp = x.pj[q % 2], x.b_pj[q % 2]
        for j in range(per):
            c = q * per + j
            col = chunk_col(c)
            for k in range(8):
                s.op("pe", lambda e, k=k, col=col, j=j, pt=pt: e.matmul(
                    pt[0:64, j * N:(j + 1) * N], g.xnT[:, k, col:col + 64], x.wt[:, k, 0:N], start=(k == 0),
                    stop=(k == 7)), reads=[x.b_wt, g.b_xn[seg_of_chunk(c)]], writes=[bp])
        evac(q * per, per, pt[0:64, 0:per * N].rearrange("p (c n) -> p c n", n=N), bp)


def ml_chunk(g, x, h, d, c, tri, state_only, first):
    s = g.s
    a = 64 * c
    if not state_only:
        s.op("pool", lambda e: e.tensor_scalar(out=x.trilf[:], in0=tri, scalar1=x.lfc[:, c:c + 1], scalar2=None,
                                               op0=ALU.mult), reads=[g.b_cst, x.b_lfc], writes=[x.b_trilf])
        s.op("pe", lambda e: e.matmul(x.pA[0:64, :], g.ones64, x.trilf[:], start=True, stop=True),
             reads=[x.b_trilf, g.b_cst], writes=[x.b_pA])
        s.op("act", lambda e: e.activation(out=x.dts[:], in_=x.pA[0:64, :], func=AF.Exp, bias=x.dbias[:, c:c + 1]),
             reads=[x.b_pA, x.b_dbias], writes=[x.b_dts])
        s.op("pe", lambda e: e.matmul(x.pB[:], x.kT[0:64, a:a + 64], x.qT[0:64, a:a + 64], start=True, stop=True),
             reads=[x.b_kT, x.b_qT], writes=[x.b_pB])
        if d == 0 and c == 32:
            g.dump("c_trilf", x.trilf[:], [64, 64], [x.b_trilf])
            g.dump("c_dts0", x.dts[:], [64, 64], [x.b_dts])
        s.op("pool", lambda e: e.tensor_tensor(out=x.dts[:], in0=x.dts[:], in1=tri, op=ALU.mult),
             reads=[x.b_dts, g.b_cst], writes=[x.b_dts])
        if d == 0 and c == 32:
            g.dump("c_dts1", x.dts[:], [64, 64], [x.b_dts])
        s.op("dve", lambda e: e.tensor_tensor(out=x.st[:], in0=x.pB[:], in1=x.dts[:], op=ALU.mult),
             reads=[x.b_pB, x.b_dts], writes=[x.b_st])
        if d == 0 and c == 32:
            g.dump("c_st", x.st[:], [64, 64], [x.b_st])
        s.op("pe", lambda e: e.matmul(x.pO[:, 0:129], x.st[:], x.v1[:, c, 0:129], start=True, stop=True),
             reads=[x.b_st, x.b_v1], writes=[x.b_pO])
        if not first:
            s.op("pe", lambda e: e.matmul(x.pO2[:, 0:129], x.qT[0:64, a:a + 64], x.Sbf[0:64, 0:129], start=True,
                                          stop=True), reads=[x.b_qT, x.b_Sbf], writes=[x.b_pO2])
            s.op("act", lambda e: e.activation(out=x.o2s[:, 0:129], in_=x.pO2[:, 0:129], func=AF.Identity,
                                               scale=x.eint[:, c:c + 1]), reads=[x.b_pO2, x.b_eint],
                 writes=[x.b_o2s])
            s.op("dve", lambda e: e.tensor_tensor(out=x.nd[:, 0:129], in0=x.pO[:, 0:129], in1=x.o2s[:, 0:129],
                                                  op=ALU.add), reads=[x.b_pO, x.b_o2s], writes=[x.b_nd])
        else:
            s.op("dve", lambda e: e.tensor_copy(out=x.nd[:, 0:129], in_=x.pO[:, 0:129]), reads=[x.b_pO],
                 writes=[x.b_nd])
        s.op("dve", lambda e: e.tensor_scalar(out=x.rden[:, 1:2], in0=x.nd[:, 128:129], scalar1=-1.0, scalar2=None,
                                              op0=ALU.mult), reads=[x.b_nd], writes=[x.b_rden])
        s.op("dve", lambda e: e.scalar_tensor_tensor(out=x.rden[:, 0:1], in0=x.nd[:, 128:129], scalar=1.0,
                                                     in1=x.rden[:, 1:2], op0=ALU.max, op1=ALU.max),
             reads=[x.b_nd, x.b_rden], writes=[x.b_rden])
        s.op("dve", lambda e: e.reciprocal(out=x.rden[:, 0:1], in_=x.rden[:, 0:1]), reads=[x.b_rden],
             writes=[x.b_rden])
        if d == 0 and c == 32:
            g.dump("c_nd", x.nd[:], [64, 132], [x.b_nd])
            g.dump("c_rden", x.rden[:], [64, 2], [x.b_rden])
        if d == 0:
            s.op("dve", lambda e: e.tensor_scalar(out=x.Hacc[:, c, :], in0=x.nd[:, 0:128], scalar1=x.rden[:, 0:1],
                                                  scalar2=None, op0=ALU.mult), reads=[x.b_nd, x.b_rden],
                 writes=[x.b_H[c]])
        else:
            s.op("dve", lambda e: e.scalar_tensor_tensor(out=x.Hacc[:, c, :], in0=x.nd[:, 0:128],
                                                         scalar=x.rden[:, 0:1], in1=x.Hacc[:, c, :], op0=ALU.mult,
                                                         op1=ALU.add), reads=[x.b_nd, x.b_rden, x.b_H[c]],
                 writes=[x.b_H[c]])
    s.op("pool", lambda e: e.tensor_scalar(out=x.kw[:, 0:64], in0=x.ktm[:, c, :], scalar1=x.what[:, c:c + 1],
                                           scalar2=None, op0=ALU.mult), reads=[x.b_ktm, x.b_what], writes=[x.b_kw])
    s.op("pe", lambda e: e.matmul(x.pU[0:64, 0:129], x.kw[:, 0:64], x.v1[:, c, 0:129], start=True, stop=True),
         reads=[x.b_kw, x.b_v1], writes=[x.b_pU])
    s.op("dve", lambda e: e.scalar_tensor_tensor(out=x.S32[0:64, 0:129], in0=x.S32[0:64, 0:129],
                                                 scalar=x.eg[:, c:c + 1], in1=x.pU[0:64, 0:129], op0=ALU.mult,
                                                 op1=ALU.add), reads=[x.b_S32, x.b_eg, x.b_pU], writes=[x.b_S32])
    s.op("act", lambda e: e.copy(out=x.Sbf[0:64, 0:129], in_=x.S32[0:64, 0:129]), reads=[x.b_S32],
         writes=[x.b_Sbf])


def hg_head(g, x, layer, h):
    s = g.s
    full = x.full
    P = 128
    segs_nohalo = [sg for sg in SEGS if sg[0] != HT0]
    if full:
        proj_fm(g, C_QI + 128 * h, 128, x.wt, x.b_wt, x.pj, x.b_pj, SEGS, x.evac_pre(128))
        conv_silu(g, x.pre, x.b_pre, 128, g.cw_hg[:, h * 9:h * 9 + 9], x.acc, x.b_acc, x.qT[:, :], x.b_qT)
    proj_fm(g, C_QI + 512 + 128 * h, 128, x.wt, x.b_wt, x.pj, x.b_pj, SEGS, x.evac_pre(128))
    conv_silu(g, x.pre, x.b_pre, 128, g.cw_hg[:, (4 + h) * 9:(4 + h) * 9 + 9], x.acc, x.b_acc, x.kT[:, :], x.b_kT)
    for q in range(NCH // 4):
        for j in range(4):
            c = q * 4 + j
            s.op("pe", lambda e, c=c, j=j: e.transpose(x.pT[0:64, j * 128:(j + 1) * 128],
                                                       x.kT[:, c * 64:c * 64 + 64], g.identb[:, :]),
                 reads=[x.b_kT, g.b_identb], writes=[x.b_pT])
        s.op("act", lambda e, q=q: e.copy(out=x.v1[:, q * 4:(q + 1) * 4, 0:128],
                                          in_=x.pT[0:64, 0:512].rearrange("p (c d) -> p c d", d=128)),
             reads=[x.b_pT], writes=[x.b_v1])
    if full:
        def evac_go(c_lo, n, p3, bp):
            s.op("act", lambda e: e.activation(out=x.gate[:, c_lo:c_lo + n, :], in_=p3, func=AF.Silu), reads=[bp],
                 writes=[x.b_gate])
        proj_tm_pair(g, x, [(C_GO + 128 * h, 128)], 4, evac_go)
    for d in range(2):
        hg_dir(g, x, layer, h, d)
    if full:
        out_stage(g, x, g.ngrep[:, 512 + 128 * h:512 + 128 * h + 128], 4 + h)


def hg_dir(g, x, layer, h, d):
    s = g.s
    full = x.full
    segs_nohalo = [sg for sg in SEGS if sg[0] != HT0]
    x.cur_dir = d
    cF = (C_FF if d == 0 else C_FB) + 128 * h
    fb_col = g.fbT[:, d * 4 + h:d * 4 + h + 1]

    def evac_f(i, a, b, pap, bp):
        a2 = a if a < 2048 else a - CX0 + 2048
        s.op("act", lambda e: e.activation(out=x.pre[:, a2:a2 + (b - a)], in_=pap, func=AF.Sigmoid, bias=fb_col),
             reads=[bp, g.b_small], writes=[x.b_pre])
    proj_fm(g, cF, 128, x.wt, x.b_wt, x.pj, x.b_pj, segs_nohalo, evac_f)
    f = x.pre[:, 0:TC]
    s.op("dve", lambda e: e.tensor_scalar(out=f, in0=f, scalar1=g.oml[:, h:h + 1], scalar2=g.lb[:, h:h + 1],
                                          op0=ALU.mult, op1=ALU.add), reads=[x.b_pre, g.b_small],
         writes=[x.b_pre])
    s.op("pool", lambda e: e.tensor_scalar(out=x.kT[:, :], in0=f, scalar1=-1.0, scalar2=1.0, op0=ALU.mult,
                                           op1=ALU.add), reads=[x.b_pre], writes=[x.b_kT])
    s.op("act", lambda e: e.activation(out=x.acc[:, 0:TC], in_=f, func=AF.Ln), reads=[x.b_pre],
         writes=[x.b_acc])
    s.op("pool", lambda e: e.memset(x.BextL[:, 0:1], 0.0), writes=[x.b_BextL])
    s.op("pool", lambda e: e.memset(x.BextC[:, 0:1], 0.0), writes=[x.b_BextC])
    s.op("dve", lambda e: e.tensor_tensor_scan(out=x.BextL[:, 1:2049], data0=x.onesr[:, 0:2048],
                                               data1=x.acc[:, 0:2048], initial=0.0, op0=ALU.mult, op1=ALU.add),
         reads=[x.b_acc, x.b_onesr], writes=[x.b_BextL])
    s.op("dve", lambda e: e.tensor_tensor_scan(out=x.BextC[:, 1:257], data0=x.onesr[:, 0:256],
                                               data1=x.acc[:, 2048:2304], initial=0.0, op0=ALU.mult,
                                               op1=ALU.add), reads=[x.b_acc, x.b_onesr], writes=[x.b_BextC])
    s.op("dve", lambda e: e.tensor_scalar(out=x.NBg[:, 0:65], in0=x.BextL[:, 0:2049:32], scalar1=-1.0,
                                          scalar2=None, op0=ALU.mult), reads=[x.b_BextL], writes=[x.b_NBg])
    s.op("dve", lambda e: e.tensor_scalar(out=x.NBg[:, 65:74], in0=x.BextC[:, 0:257:32], scalar1=-1.0,
                                          scalar2=None, op0=ALU.mult), reads=[x.b_BextC], writes=[x.b_NBg])
    s.op("dve", lambda e: e.tensor_tensor(out=x.EG[:, 0:32], in0=x.BextL[:, 64:2049:64],
                                          in1=x.BextL[:, 0:2048:64], op=ALU.subtract), reads=[x.b_BextL],
         writes=[x.b_EG])
    s.op("dve", lambda e: e.tensor_tensor(out=x.EG[:, 32:36], in0=x.BextC[:, 64:257:64],
                                          in1=x.BextC[:, 0:256:64], op=ALU.subtract), reads=[x.b_BextC],
         writes=[x.b_EG])
    s.op("act", lambda e: e.activation(out=x.EG[:], in_=x.EG[:], func=AF.Exp), reads=[x.b_EG], writes=[x.b_EG])
    ctxc, latc = chunk_order(d)
    base = 8 * ML_W + (h * 2 + d) * HG_W
    s.op("dve", lambda e: e.memset(x.S32[:], 0.0), writes=[x.b_S32])
    s.op("pool", lambda e: e.memset(x.Sbf[:], 0.0), writes=[x.b_Sbf])
    if not full:
        for c in latc:
            hg_chunk(g, x, h, d, c, state_only=True, first=False)
        s.op("act", lambda e: e.activation(out=x.S32[:, 128:129], in_=x.BextL[:, 2048:2049], func=AF.Exp),
             reads=[x.b_BextL], writes=[x.b_S32])
        write_summary(g, x, 128, 128, base)
        return
    for i, c in enumerate(ctxc):
        hg_chunk(g, x, h, d, c, state_only=False, first=(i == 0))
    fold_state(g, x, 128, 128, base)
    for c in latc:
        hg_chunk(g, x, h, d, c, state_only=False, first=False)


def hg_chunk(g, x, h, d, c, state_only, first):
    s = g.s
    a = 64 * c
    tri = g.triF if d == 0 else g.triB
    stm, b_stm = (x.stF, x.b_stF) if d == 0 else (x.stB, x.b_stB)
    if c < 32:
        B, bB, cl, nb0 = x.BextL, x.b_BextL, c, 0
    else:
        B, bB, cl, nb0 = x.BextC, x.b_BextC, c - 32, 65
    c0 = 64 * cl
    r = c0 + 32

    def nb(idx):
        return x.NBg[:, nb0 + idx // 32:nb0 + idx // 32 + 1]

    def pb(idx):
        return B[:, idx:idx + 1]
    if d == 0:
        src = B[:, c0 + 1:c0 + 65]
        specs = [(1.0, nb(r)), (-1.0, pb(r)), (1.0, nb(c0)), (-1.0, pb(c0 + 64))]
    else:
        src = B[:, c0:c0 + 64]
        specs = [(-1.0, pb(r)), (1.0, nb(r)), (-1.0, pb(c0 + 64)), (1.0, nb(c0))]
    need = [1, 3] if state_only else [0, 1, 2, 3]
    for i in need:
        if state_only and i == 1:
            continue
        sc, bias = specs[i]
        s.op("act", lambda e, i=i, sc=sc, bias=bias: e.activation(out=x.tmpA[:, i, :], in_=src, func=AF.Exp,
                                                                  scale=sc, bias=bias), reads=[bB, x.b_NBg],
             writes=[x.b_tmpA])
    if not state_only:
        s.op("dve", lambda e: e.tensor_tensor(out=x.tmpB[:, 0, :], in0=x.qT[:, a:a + 64], in1=x.tmpA[:, 0, :],
                                              op=ALU.mult), reads=[x.b_qT, x.b_tmpA], writes=[x.b_tmpB])
        s.op("pool", lambda e: e.tensor_tensor(out=x.tmpB[:, 1, :], in0=x.kT[:, a:a + 64], in1=x.tmpA[:, 1, :],
                                               op=ALU.mult), reads=[x.b_kT, x.b_tmpA], writes=[x.b_tmpB])
        s.op("dve", lambda e: e.tensor_tensor(out=x.tmpB[:, 2, :], in0=x.qT[:, a:a + 64], in1=x.tmpA[:, 2, :],
                                              op=ALU.mult), reads=[x.b_qT, x.b_tmpA], writes=[x.b_tmpB])
    s.op("pool", lambda e: e.tensor_tensor(out=x.tmpB[:, 3, :], in0=x.kT[:, a:a + 64], in1=x.tmpA[:, 3, :],
                                           op=ALU.mult), reads=[x.b_kT, x.b_tmpA], writes=[x.b_tmpB])
    if not state_only:
        s.op("pe", lambda e: e.matmul(x.pB[:], x.tmpB[:, 1, :], x.tmpB[:, 0, :], start=True, stop=True),
             reads=[x.b_tmpB], writes=[x.b_pB])
        s.op("dve", lambda e: e.copy_predicated(out=stm[:], mask=tri.bitcast(mybir.dt.uint32), data=x.pB[:]),
             reads=[x.b_pB, g.b_cst], writes=[b_stm])
    s.op("pe", lambda e: e.transpose(x.pT[0:64, 0:128], x.tmpB[:, 3, :], g.identb[:, :]),
         reads=[x.b_tmpB, g.b_identb], writes=[x.b_pT])
    s.op("act", lambda e: e.copy(out=x.kw[:, 0:128], in_=x.pT[0:64, 0:128]), reads=[x.b_pT], writes=[x.b_kw])
    if not state_only:
        s.op("pe", lambda e: e.matmul(x.pO[:, 0:128], stm[:], x.v1[:, c, 0:128], start=True, stop=first),
             reads=[b_stm, x.b_v1], writes=[x.b_pO])
        if not first:
            s.op("pe", lambda e: e.matmul(x.pO[:, 0:128], x.tmpB[:, 2, :], x.Sbf[:, 0:128], start=False, stop=True),
                 reads=[x.b_tmpB, x.b_Sbf], writes=[x.b_pO])
        if d == 0:
            s.op("act", lambda e: e.copy(out=x.Hacc[:, c, :], in_=x.pO[:, 0:128]), reads=[x.b_pO],
                 writes=[x.b_H[c]])
        else:
            s.op("dve", lambda e: e.tensor_tensor(out=x.Hacc[:, c, :], in0=x.pO[:, 0:128], in1=x.Hacc[:, c, :],
                                                  op=ALU.add), reads=[x.b_pO, x.b_H[c]], writes=[x.b_H[c]])
    s.op("pe", lambda e: e.matmul(x.pU[:, 0:128], x.kw[:, 0:128], x.v1[:, c, 0:128], start=True, stop=True),
         reads=[x.b_kw, x.b_v1], writes=[x.b_pU])
    s.op("dve", lambda e: e.scalar_tensor_tensor(out=x.S32[:, 0:128], in0=x.S32[:, 0:128],
                                                 scalar=x.EG[:, c:c + 1], in1=x.pU[:, 0:128], op0=ALU.mult,
                                                 op1=ALU.add), reads=[x.b_S32, x.b_EG, x.b_pU], writes=[x.b_S32])
    s.op("act", lambda e: e.copy(out=x.Sbf[:, 0:128], in_=x.S32[:, 0:128]), reads=[x.b_S32], writes=[x.b_Sbf])


NE = 32
LIM = 7.0
ALPHA = 1.702


def ffn_phase(g, layer, last, n_exp=NE):
    nc, s = g.nc, g.s
    din, dout = g.din, g.dout
    w_out = din("w_out", [D, D]).rearrange("(k p) f -> p k f", p=128)
    router_w = din("router_w", [D, NE]).rearrange("(k p) e -> p k e", p=128)
    router_b = din("router_b", [1, NE])
    w_gu = din("w_gu", [NE, D, 2 * D])
    b_guT = din("b_guT", [128, NE, 16])
    w_down = din("w_down", [NE, D, D])
    b_down = din("b_down", [NE, D])
    selm = din("selm", [NE, NE * 128])
    xT_v = g.xT_d.rearrange("(k p) t -> p k t", p=128)
    if last:
        outT = dout("outT", [D, NLAT])
        out_v = outT.rearrange("(k p) t -> p k t", p=128)
        groups = [[(0, 512, 0, 0), (512, 1024, 512, 0)], [(1024, 1536, 1024, 0), (1536, 2048, 1536, 0)]]
    else:
        out_v = g.x1T.rearrange("(k p) t -> p k t", p=128)
        groups = [[(0, 512, 0, 0), (512, 1024, 512, 0), (2048, 2304, CX0, 1)],
                  [(1024, 1536, 1024, 0), (1536, 2048, 1536, 0)]]
    NG = 1280
    with ExitStack() as e2:
        x = Ctx()

        def sb(name, shape, dt=F32):
            t = e2.enter_context(nc.sbuf_tensor(PFX[0] + "f_" + name, list(shape), dt))
            setattr(x, name, t)
            setattr(x, "b_" + name, Buf(name))
            return t

        def ps(name, shape, dt=F32):
            t = e2.enter_context(nc.psum_tensor(PFX[0] + "f_" + name, list(shape), dt))
            setattr(x, name, t)
            setattr(x, "b_" + name, Buf(name))
            return t
        sb("xres", [128, 8, NG]); sb("h2T", [128, 8, NG], BF16); sb("AT", [128, 8, NG], BF16)
        x.b_xr = [Buf("xr%d" % j) for j in range(8)]
        sb("GT", [NE, NG], BF16)
        wgus = [sb("wgu0", [128, 8, 2048], BF16), sb("wgu1", [128, 8, 2048], BF16)]
        b_wgus = [x.b_wgu0, x.b_wgu1]
        sb("wd", [128, 8, 1024], BF16)
        x.wo, x.b_wo = x.wd, x.b_wd
        x.yt, x.b_yt = x.AT, x.b_AT
        sb("rw", [128, 8, NE]); sb("rb", [1, NE]); sb("bgu", [128, NE, 16])
        sb("bd", [NE, D], BF16); sb("onesb", [NE, 128], BF16); sb("gsel", [NE, 512], BF16)
        sb("sq", [128, 8, 512]); sb("rs", [128, 512])
        gms = [sb("gm0", [128, 512]), sb("gm1", [128, 512])]; b_gms = [x.b_gm0, x.b_gm1]
        sgs = [sb("sg0", [128, 512]), sb("sg1", [128, 512])]; b_sgs = [x.b_sg0, x.b_sg1]
        lts = [sb("lt0", [128, 512]), sb("lt1", [128, 512])]; b_lts = [x.b_lt0, x.b_lt1]
        a1s = [sb("a10", [128, 512]), sb("a11", [128, 512])]; b_a1s = [x.b_a10, x.b_a11]
        s.op("pool", lambda e: e.memset(x.onesb[:], 1.0), writes=[x.b_onesb])
        sb("lg", [128, NE]); sb("m8", [128, 8]); sb("msk", [128, NE]); sb("ex", [128, NE]); sb("rsum", [128, 2])
        sb("gtm", [128, NE])
        pu = [ps("pg0", [128, 512]), ps("pl0", [128, 512]), ps("pg1", [128, 512]), ps("pl1", [128, 512])]
        b_pu = [x.b_pg0, x.b_pl0, x.b_pg1, x.b_pl1]
        ps("pgate", [128, 512])
        pdn = [ps("pd0", [128, 512]), ps("pd1", [128, 512])]
        b_pdn = [x.b_pd0, x.b_pd1]
        ps("psm", [128, 512])
        for dst, src, bb in [(x.rw[:], router_w, x.b_rw), (x.rb[:], router_b, x.b_rb), (x.bgu[:], b_guT, x.b_bgu)]:
            s.dma("sp", lambda e, dst=dst, src=src: e.dma_start(out=dst, in_=src), writes=[bb])
        s.dma("pool", lambda e: e.dma_start(out=x.bd[:], in_=b_down), writes=[x.b_bd])
        G1 = lambda j, mc: g.mod[:, 16 + j, mc:mc + 1]
        G2 = lambda j, mc: g.mod[:, 40 + j, mc:mc + 1]
        w_gu_v = w_gu.rearrange("e (k p) f -> e p k f", p=128)
        w_down_v = w_down.rearrange("e (k p) f -> e p k f", p=128)
        ncast = [0]

        def load_gu(e):
            s.dma("pool", lambda e_: e_.dma_start(out=wgus[e % 2][:], in_=w_gu_v[e]), writes=[b_wgus[e % 2]])

        def load_d(e):
            s.dma("pool", lambda e_: e_.dma_start(out=x.wd[:], in_=w_down_v[e]), writes=[x.b_wd])

        def up_fc(e, a, b, w, fc, wgu, b_wgu):
            pg, bpg = pu[(fc % 2) * 2], b_pu[(fc % 2) * 2]
            gm, b_gm, sg, b_sg = gms[fc % 2], b_gms[fc % 2], sgs[fc % 2], b_sgs[fc % 2]
            lt, b_lt, a1, b_a1 = lts[fc % 2], b_lts[fc % 2], a1s[fc % 2], b_a1s[fc % 2]
            pl, bpl = pu[(fc % 2) * 2 + 1], b_pu[(fc % 2) * 2 + 1]
            for k in range(8):
                s.op("pe", lambda e_, k=k, fc=fc, a=a, b=b, w=w, pg=pg: e_.matmul(
                    pg[:, 0:w], wgu[:, k, fc * 128:(fc + 1) * 128], x.h2T[:, k, a:b], start=(k == 0),
                    stop=(k == 7)), reads=[b_wgu, x.b_h2T], writes=[bpg])
            for k in range(8):
                s.op("pe", lambda e_, k=k, fc=fc, a=a, b=b, w=w, pl=pl: e_.matmul(
                    pl[:, 0:w], wgu[:, k, 1024 + fc * 128:1024 + (fc + 1) * 128], x.h2T[:, k, a:b],
                    start=(k == 0), stop=(k == 7)), reads=[b_wgu, x.b_h2T], writes=[bpl])
            s.op("dve", lambda e_, e=e, fc=fc, w=w, pg=pg: e_.tensor_scalar(
                out=gm[:, 0:w], in0=pg[:, 0:w], scalar1=x.bgu[:, e, fc:fc + 1], scalar2=LIM, op0=ALU.add,
                op1=ALU.min), reads=[bpg, x.b_bgu], writes=[b_gm])
            s.op("act", lambda e_, w=w: e_.activation(out=sg[:, 0:w], in_=gm[:, 0:w], func=AF.Sigmoid,
                                                      scale=ALPHA), reads=[b_gm], writes=[b_sg])
            s.op("act", lambda e_, e=e, fc=fc, w=w, pl=pl: e_.activation(
                out=lt[:, 0:w], in_=pl[:, 0:w], func=AF.Identity, bias=x.bgu[:, e, 8 + fc:9 + fc]),
                reads=[bpl, x.b_bgu], writes=[b_lt])
            s.op("pool", lambda e_, w=w: e_.tensor_scalar(out=lt[:, 0:w], in0=lt[:, 0:w], scalar1=LIM,
                                                          scalar2=-LIM, op0=ALU.min, op1=ALU.max),
                 reads=[b_lt], writes=[b_lt])
            s.op("pool", lambda e_, w=w: e_.tensor_tensor(out=a1[:, 0:w], in0=gm[:, 0:w],
                                                          in1=sg[:, 0:w], op=ALU.mult),
                 reads=[b_gm, b_sg], writes=[b_a1])
            s.op("dve", lambda e_, w=w: e_.scalar_tensor_tensor(
                out=a1[:, 0:w], in0=lt[:, 0:w], scalar=1.0, in1=a1[:, 0:w], op0=ALU.add,
                op1=ALU.mult), reads=[b_lt, b_a1], writes=[b_a1])
            s.op("dve", lambda e_, fc=fc, a=a, b=b, w=w: e_.tensor_tensor(
                out=x.AT[:, fc, a:b], in0=a1[:, 0:w], in1=x.pgate[:, 0:w], op=ALU.mult),
                reads=[b_a1, x.b_pgate], writes=[x.b_AT])

        for gi, segs in enumerate(groups):
            loc = []
            off = 0
            for (t0, t1, xc, mc) in segs:
                loc.append((off, off + (t1 - t0), t0, xc, mc))
                off += t1 - t0
            ntok = off
            s.dma("pool", lambda e: e.dma_start(out=x.wo[:], in_=w_out), writes=[x.b_wo])
            for si, (l0, l1, t0, xc, mc) in enumerate(loc):
                w = l1 - l0
                st, bs = x.sq, x.b_sq
                s.dma("sp", lambda e, st=st, xc=xc, w=w: e.dma_start(out=st[:, :, 0:w], in_=xT_v[:, :, xc:xc + w]),
                      writes=[bs])
                s.dma("sp", lambda e, t0=t0, w=w: e.dma_start(out=x.yt[:, :, 0:w], in_=g.yT_d[:, :, t0:t0 + w]),
                      reads=[g.b_yTd], writes=[x.b_yt])
                for j in range(8):
                    pt, bp = pdn[j % 2], b_pdn[j % 2]
                    for k in range(8):
                        s.op("pe", lambda e, j=j, k=k, pt=pt, t0=t0, w=w: e.matmul(
                            pt[:, 0:w], x.wo[:, k, j * 128:(j + 1) * 128], x.yt[:, k, 0:w], start=(k == 0),
                            stop=(k == 7)), reads=[x.b_wo, x.b_yt], writes=[bp])
                    s.op("dve", lambda e, j=j, pt=pt, st=st, l0=l0, l1=l1, w=w, mc=mc: e.scalar_tensor_tensor(
                        out=x.xres[:, j, l0:l1], in0=pt[:, 0:w], scalar=G1(j, mc), in1=st[:, j, 0:w], op0=ALU.mult,
                        op1=ALU.add), reads=[bp, bs, g.b_mod], writes=[x.b_xr[j]])
                s.op("act", lambda e, l0=l0, l1=l1, w=w: e.activation(out=x.sq[:, :, 0:w], in_=x.xres[:, :, l0:l1],
                                                                      func=AF.Square), reads=x.b_xr,
                     writes=[x.b_sq])
                for k in range(8):
                    s.op("pe", lambda e, k=k, w=w: e.matmul(x.psm[:, 0:w], g.ones, x.sq[:, k, 0:w], start=(k == 0),
                                                            stop=(k == 7)), reads=[x.b_sq, g.b_cst],
                         writes=[x.b_psm])
                s.op("act", lambda e, w=w: e.activation(out=x.rs[:, 0:w], in_=x.psm[:, 0:w], func=AF.Sqrt,
                                                        scale=1.0 / D, bias=EPS), reads=[x.b_psm], writes=[x.b_rs])
                s.op("dve", lambda e, w=w: e.reciprocal(out=x.rs[:, 0:w], in_=x.rs[:, 0:w]), reads=[x.b_rs],
                     writes=[x.b_rs])
                for k in range(8):
                    s.op("dve", lambda e, k=k, w=w, l0=l0, l1=l1, mc=mc: e.scalar_tensor_tensor(
                        out=x.sq[:, k, 0:w], in0=x.xres[:, k, l0:l1], scalar=g.A2[:, k, mc:mc + 1], in1=x.rs[:, 0:w],
                        op0=ALU.mult, op1=ALU.mult), reads=[x.b_xr[k], x.b_rs, g.b_A], writes=[x.b_sq])
                    s.op("act", lambda e, k=k, w=w, mc=mc: e.activation(
                        out=x.sq[:, k, 0:w], in_=x.sq[:, k, 0:w], func=AF.Identity, bias=g.mod[:, 24 + k, mc:mc + 1]),
                        reads=[x.b_sq, g.b_mod], writes=[x.b_sq])
                s.op("pool", lambda e, w=w, l0=l0, l1=l1: e.tensor_copy(out=x.h2T[:, :, l0:l1], in_=x.sq[:, :, 0:w]),
                     reads=[x.b_sq], writes=[x.b_h2T])
                for q in range(w // 128):
                    for k in range(8):
                        s.op("pe", lambda e, k=k, q=q: e.matmul(x.pgate[:, 0:NE], x.sq[:, k, q * 128:(q + 1) * 128],
                                                                x.rw[:, k, :], start=(k == 0), stop=False),
                             reads=[x.b_sq, x.b_rw], writes=[x.b_pgate])
                    s.op("pe", lambda e: e.matmul(x.pgate[:, 0:NE], g.ones[0:1, :], x.rb[:], start=False, stop=True),
                         reads=[x.b_rb, g.b_cst], writes=[x.b_pgate])
                    s.op("dve", lambda e: e.tensor_copy(out=x.lg[:], in_=x.pgate[:, 0:NE]), reads=[x.b_pgate],
                         writes=[x.b_lg])
                    s.op("dve", lambda e: e.max(out=x.m8[:], in_=x.lg[:]), reads=[x.b_lg], writes=[x.b_m8])
                    s.op("dve", lambda e: e.tensor_scalar(out=x.msk[:], in0=x.lg[:], scalar1=x.m8[:, 3:4],
                                                          scalar2=None, op0=ALU.is_ge), reads=[x.b_lg, x.b_m8],
                         writes=[x.b_msk])
                    s.op("dve", lambda e: e.tensor_scalar(out=x.rsum[:, 0:1], in0=x.m8[:, 0:1], scalar1=-1.0,
                                                          scalar2=None, op0=ALU.mult), reads=[x.b_m8],
                         writes=[x.b_rsum])
                    s.op("act", lambda e: e.activation(out=x.ex[:], in_=x.lg[:], func=AF.Exp, bias=x.rsum[:, 0:1]),
                         reads=[x.b_lg, x.b_rsum], writes=[x.b_ex])
                    s.op("dve", lambda e: e.tensor_tensor(out=x.ex[:], in0=x.ex[:], in1=x.msk[:], op=ALU.mult),
                         reads=[x.b_ex, x.b_msk], writes=[x.b_ex])
                    s.op("dve", lambda e: e.tensor_reduce(out=x.rsum[:, 1:2], in_=x.ex[:], axis=AX.X, op=ALU.add),
                         reads=[x.b_ex], writes=[x.b_rsum])
                    s.op("dve", lambda e: e.reciprocal(out=x.rsum[:, 1:2], in_=x.rsum[:, 1:2]), reads=[x.b_rsum],
                         writes=[x.b_rsum])
                    s.op("dve", lambda e: e.tensor_scalar(out=x.gtm[:], in0=x.ex[:], scalar1=x.rsum[:, 1:2],
                                                          scalar2=None, op0=ALU.mult), reads=[x.b_ex, x.b_rsum],
                         writes=[x.b_gtm])
                    s.op("pe", lambda e: e.transpose(x.pgate[0:NE, 128:256], x.gtm[:], g.ident), reads=[x.b_gtm, g.b_cst],
                         writes=[x.b_pgate])
                    s.op("act", lambda e, q=q, l0=l0: e.copy(out=x.GT[:, l0 + q * 128:l0 + (q + 1) * 128],
                                                             in_=x.pgate[0:NE, 128:256]), reads=[x.b_pgate],
                         writes=[x.b_GT])
            if gi == 0 and "GT" in g.dumps:
                g.dump("GT", x.GT[:], [NE, NG], [x.b_GT])
                g.dump("xres", x.xres[:], [128, 8, NG], x.b_xr)
            tiles = [(a, min(a + 512, ntok)) for a in range(0, ntok, 512)]
            mc_of = {}
            for (l0, l1, t0, xc, mc) in loc:
                for a in range(l0, l1, 128):
                    mc_of[a] = mc
            if n_exp > 0:
                load_gu(0)
            for e in range(n_exp):
                load_d(e)
                if e + 1 < n_exp:
                    load_gu(e + 1)
                wgu, b_wgu = wgus[e % 2], b_wgus[e % 2]
                for (a, b) in tiles:
                    w = b - a
                    s.op("pool", lambda e_, e=e, a=a, b=b, w=w: e_.tensor_scalar(
                        out=x.gsel[:, 0:w], in0=x.GT[:, a:b], scalar1=g.identb[0:NE, e:e + 1], scalar2=None,
                        op0=ALU.mult), reads=[x.b_GT, g.b_identb], writes=[x.b_gsel])
                    s.op("pe", lambda e_, w=w: e_.matmul(x.pgate[:, 0:w], x.onesb[:], x.gsel[:, 0:w], start=True,
                                                         stop=True), reads=[x.b_onesb, x.b_gsel],
                         writes=[x.b_pgate])
                    for fc in range(8):
                        up_fc(e, a, b, w, fc, wgu, b_wgu)
                for (a, b) in tiles:
                    w = b - a
                    for j in range(8):
                        pt, bp = pdn[j % 2], b_pdn[j % 2]
                        for fk in range(8):
                            s.op("pe", lambda e_, j=j, fk=fk, a=a, b=b, w=w, pt=pt: e_.matmul(
                                pt[:, 0:w], x.wd[:, fk, j * 128:(j + 1) * 128], x.AT[:, fk, a:b], start=(fk == 0),
                                stop=(fk == 7)), reads=[x.b_wd, x.b_AT], writes=[bp])
                        for sub in range(a, b, 128):
                            pass
                        runs = []
                        for sub in range(a, b, 128):
                            mc = mc_of[sub]
                            if runs and runs[-1][2] == mc:
                                runs[-1][1] = sub + 128
                            else:
                                runs.append([sub, sub + 128, mc])
                        for (r0, r1, mc) in runs:
                            s.op("dve", lambda e_, j=j, pt=pt, r0=r0, r1=r1, a=a, mc=mc: e_.scalar_tensor_tensor(
                                out=x.xres[:, j, r0:r1], in0=pt[:, r0 - a:r1 - a], scalar=G2(j, mc),
                                in1=x.xres[:, j, r0:r1], op0=ALU.mult, op1=ALU.add),
                                reads=[bp, g.b_mod, x.b_xr[j]], writes=[x.b_xr[j]])
            for (a, b) in tiles:
                w = b - a
                for j in range(8):
                    pt, bp = pdn[j % 2], b_pdn[j % 2]
                    s.op("pe", lambda e_, j=j, a=a, b=b, w=w, pt=pt: e_.matmul(
                        pt[:, 0:w], x.bd[:, j * 128:(j + 1) * 128], x.GT[:, a:b], start=True, stop=True),
                        reads=[x.b_bd, x.b_GT], writes=[bp])
                    runs = []
                    for sub in range(a, b, 128):
                        mc = mc_of[sub]
                        if runs and runs[-1][2] == mc:
                            runs[-1][1] = sub + 128
                        else:
                            runs.append([sub, sub + 128, mc])
                    for (r0, r1, mc) in runs:
                        s.op("dve", lambda e_, j=j, pt=pt, r0=r0, r1=r1, a=a, mc=mc: e_.scalar_tensor_tensor(
                            out=x.xres[:, j, r0:r1], in0=pt[:, r0 - a:r1 - a], scalar=G2(j, mc),
                            in1=x.xres[:, j, r0:r1], op0=ALU.mult, op1=ALU.add),
                            reads=[bp, g.b_mod, x.b_xr[j]], writes=[x.b_xr[j]])
            for (l0, l1, t0, xc, mc) in loc:
                w = l1 - l0
                if not last:
                    s.dma("sp", lambda e, l0=l0, l1=l1, xc=xc, w=w: e.dma_start(out=out_v[:, :, xc:xc + w],
                                                                              in_=x.xres[:, :, l0:l1]),
                          reads=x.b_xr)
                else:
                    s.op("act", lambda e, l0=l0, l1=l1, w=w: e.activation(out=x.sq[:, :, 0:w],
                                                                          in_=x.xres[:, :, l0:l1], func=AF.Square),
                         reads=x.b_xr, writes=[x.b_sq])
                    for k in range(8):
                        s.op("pe", lambda e, k=k, w=w: e.matmul(x.psm[:, 0:w], g.ones, x.sq[:, k, 0:w],
                                                                start=(k == 0), stop=(k == 7)),
                             reads=[x.b_sq, g.b_cst], writes=[x.b_psm])
                    s.op("act", lambda e, w=w: e.activation(out=x.rs[:, 0:w], in_=x.psm[:, 0:w], func=AF.Sqrt,
                                                            scale=1.0 / D, bias=EPS), reads=[x.b_psm],
                         writes=[x.b_rs])
                    s.op("dve", lambda e, w=w: e.reciprocal(out=x.rs[:, 0:w], in_=x.rs[:, 0:w]), reads=[x.b_rs],
                         writes=[x.b_rs])
                    for k in range(8):
                        s.op("dve", lambda e, k=k, w=w, l0=l0, l1=l1: e.scalar_tensor_tensor(
                            out=x.sq[:, k, 0:w], in0=x.xres[:, k, l0:l1], scalar=g.ngt[:, 2, k:k + 1],
                            in1=x.rs[:, 0:w], op0=ALU.mult, op1=ALU.mult), reads=[x.b_xr[k], x.b_rs, g.b_ngt],
                            writes=[x.b_sq])
                    s.dma("sp", lambda e, t0=t0, w=w: e.dma_start(out=out_v[:, :, t0:t0 + w], in_=x.sq[:, :, 0:w]),
                          reads=[x.b_sq], is_out=True)
        s.barrier()


D=1024; NLAT=2048; TT=2432

def make_consts():
    c = np.zeros((128, 384), np.float32)
    c[:, 0:128] = np.eye(128, dtype=np.float32)
    c[:, 128:256] = 1.0
    k = np.arange(64)[:, None]; n = np.arange(64)[None, :]
    tf = (k <= n).astype(np.float32); tb = (k >= n).astype(np.float32)
    c[0:64, 256:320] = tf; c[64:128, 256:320] = tf
    c[0:64, 320:384] = tb; c[64:128, 320:384] = tb
    return c

def core_xT(x, ctx, core):
    b, sg = core // 4, core % 4
    t0 = sg * NLAT
    out = np.zeros((TT, D), np.float32)
    out[0:2048] = x[b, t0:t0 + NLAT]
    if sg > 0:
        out[2048:2112] = x[b, t0 - 64:t0]
    if sg < 3:
        out[2112:2176] = x[b, t0 + NLAT:t0 + NLAT + 64]
    out[2176:2432] = ctx[b]
    return np.ascontiguousarray(out.T)

def fm_cols(v):
    return np.ascontiguousarray(v.reshape(-1, 128).T)

def mixer_inputs(inp, l, core):
    b, sg = core // 4, core % 4
    m = {}
    fl = np.zeros((128, 4), np.float32)
    fl[:, 0] = 1.0 if sg > 0 else 0.0
    fl[:, 1] = 1.0 if sg < 3 else 0.0
    m['flags'] = fl
    sel = np.zeros((128, 16), np.float32)
    for j in range(8):
        if j // 4 == b and j % 4 < sg: sel[:, j] = 1.0
        if j // 4 == b and j % 4 > sg: sel[:, 8 + j] = 1.0
    m['sel'] = sel
    m['w_in'] = inp['w_in'][l]
    cw = inp['mlstm_conv'][l].reshape(9, 512)
    cw_ml = np.zeros((64, 8, 9), np.float32)
    for which in range(2):
        for h in range(4):
            ch0 = which * 256 + 64 * h
            cw_ml[:, which * 4 + h, :] = cw[:, ch0:ch0 + 64].T
    m['cw_ml'] = cw_ml
    gb = inp['mlstm_gate_b'][l].reshape(16)
    m['gbrep'] = np.ascontiguousarray(np.broadcast_to(gb[None, None, :], (64, 36, 16))).astype(np.float32)
    m['mlng'] = np.ascontiguousarray(np.broadcast_to(inp['mlstm_norm_g'][l][None, :], (64, 512))).astype(np.float32)
    ch = inp['hgrn_conv'][l].reshape(9, 1024)
    cw_hg = np.zeros((128, 8, 9), np.float32)
    for which in range(2):
        for h in range(4):
            ch0 = which * 512 + 128 * h
            cw_hg[:, which * 4 + h, :] = ch[:, ch0:ch0 + 128].T
    m['cw_hg'] = cw_hg
    fb = inp['hgrn_f_b'][l]
    m['fbT'] = np.ascontiguousarray(fb.reshape(2, 4, 128).transpose(2, 0, 1)).astype(np.float32)
    m['lbraw'] = np.ascontiguousarray(inp['hgrn_lb_raw'].reshape(2, 4, 128).transpose(2, 0, 1)).astype(np.float32)
    m['hgng'] = np.ascontiguousarray(np.broadcast_to(inp['hgrn_norm_g'][l][None, :], (64, 512))).astype(np.float32)
    return m

def ffn_inputs(inp, l):
    m = {}
    m['w_out'] = inp['w_out'][l]
    m['router_w'] = inp['router_w'][l]
    m['router_b'] = np.ascontiguousarray(inp['router_b'][l][None, :])
    m['w_gu'] = inp['w_gu'][l]
    m['b_guT'] = np.ascontiguousarray(inp['b_gu'][l].reshape(32, 16, 128).transpose(2, 0, 1))
    m['w_down'] = inp['w_down'][l]
    m['b_down'] = inp['b_down'][l]
    sel = np.zeros((32, 32, 128), np.float32)
    for e in range(32):
        sel[e, e, :] = 1.0
    m['selm'] = sel.reshape(32, 32 * 128)
    return m

def base_inputs(inp, l, core, x_cur, ctx_cur):
    b = core // 4
    m = {}
    m['xT'] = core_xT(x_cur, ctx_cur, core)
    cT = np.stack([fm_cols(inp['c'][b]), fm_cols(inp['c_ctx'])], axis=-1)
    m['cT'] = np.ascontiguousarray(cT)
    m['consts'] = make_consts()
    m['w_ada'] = inp['w_ada'][l]
    m['b_adaT'] = fm_cols(inp['b_ada'][l])
    m['ng'] = np.ascontiguousarray(np.stack([fm_cols(inp['norm1_g'][l]), fm_cols(inp['norm2_g'][l]), fm_cols(inp['final_g'])], axis=1))
    return m


def _allgather(g, src_d, dst_d, b_src, b_dst):
    g.s.dma("pool", lambda e: e.collective_compute("AllGather", mybir.AluOpType.bypass,
                                                    replica_groups=[list(range(8))], ins=[src_d], outs=[dst_d]),
            reads=[b_src], writes=[b_dst], inc=1)


def _halo_exchange(g):
    nc, s = g.nc, g.s
    PFX[0] = "HX_"
    hsel_d = nc.dram_tensor("hsel", [128, 16], F32, kind="ExternalInput").ap()
    hb_d = nc.dram_tensor("hb_d", [128, 1024], F32, kind="Internal").ap()
    hg_d = nc.dram_tensor("hg_d", [8 * 128, 1024], F32, kind="Internal").ap()
    x1v = g.x1T.rearrange("(k p) t -> p k t", p=128)
    with ExitStack() as e2:
        hb = e2.enter_context(nc.sbuf_tensor("HX_hb", [128, 8, 128], F32)); b_hb = Buf("hb")
        hg = e2.enter_context(nc.sbuf_tensor("HX_hg", [128, 8, 1024], F32)); b_hg = Buf("hg")
        hs = e2.enter_context(nc.sbuf_tensor("HX_hs", [128, 16], F32)); b_hs = Buf("hs")
        ha = e2.enter_context(nc.sbuf_tensor("HX_ha", [128, 8, 128], F32)); b_ha = Buf("ha")
        b_hbd, b_hgd = Buf("hbd"), Buf("hgd")
        s.dma("sp", lambda e: e.dma_start(out=hs[:], in_=hsel_d), writes=[b_hs])
        s.dma("sp", lambda e: e.dma_start(out=hb[:, :, 0:64], in_=x1v[:, :, 0:64]), writes=[b_hb])
        s.dma("sp", lambda e: e.dma_start(out=hb[:, :, 64:128], in_=x1v[:, :, NLAT - 64:NLAT]), writes=[b_hb])
        s.dma("sp", lambda e: e.dma_start(out=hb_d.rearrange("p (k c) -> p k c", c=128), in_=hb[:]), reads=[b_hb],
              writes=[b_hbd])
        _allgather(g, hb_d, hg_d, b_hbd, b_hgd)
        s.dma("sp", lambda e: e.dma_start(out=hg[:], in_=hg_d.rearrange("(j p) c -> p j c", p=128)), reads=[b_hgd],
              writes=[b_hg])
        s.op("dve", lambda e: e.memset(ha[:], 0.0), writes=[b_ha])
        for j in range(8):
            gj = hg[:, j, :].rearrange("p (k c) -> p k c", c=128)
            s.op("dve", lambda e, gj=gj, j=j: e.scalar_tensor_tensor(out=ha[:, :, 0:64], in0=gj[:, :, 64:128],
                                                                     scalar=hs[:, j:j + 1], in1=ha[:, :, 0:64],
                                                                     op0=ALU.mult, op1=ALU.add),
                 reads=[b_hg, b_hs, b_ha], writes=[b_ha])
            s.op("dve", lambda e, gj=gj, j=j: e.scalar_tensor_tensor(out=ha[:, :, 64:128], in0=gj[:, :, 0:64],
                                                                     scalar=hs[:, 8 + j:9 + j], in1=ha[:, :, 64:128],
                                                                     op0=ALU.mult, op1=ALU.add),
                 reads=[b_hg, b_hs, b_ha], writes=[b_ha])
        s.dma("sp", lambda e: e.dma_start(out=x1v[:, :, HT0:HT0 + 128], in_=ha[:]), reads=[b_ha])
        s.barrier()


def _build_fused(n_exp=32, layers=(0, 1), stages=("summ", "full")):
    g = None
    for l in layers:
        last = l == 1
        for stage in stages:
            xsrc = None if l == 0 else g.x1T
            g = build(l, last, stage, set(), g=g, xT_src=xsrc)
            if l == 0 and stage == "summ":
                g.x1T = g.nc.dram_tensor("x1T", [D, TT], F32, kind="Internal").ap()
            if stage == "summ":
                g.summ_d = g.nc.dram_tensor("summ_d%d" % l, [128, NS], F32, kind="Internal").ap()
                g.gath_d = g.nc.dram_tensor("gath_d%d" % l, [8 * 128, NS], F32, kind="Internal").ap()
                g.summ_all_v = g.gath_d.rearrange("(j p) c -> j p c", p=128)
                g.b_summd, g.b_gath = Buf("summd"), Buf("gath")
            g.ml_heads = [0, 1, 2, 3]
            g.hg_heads = [0, 1, 2, 3]
            mixer_setup(g, l)
            mixer_phase(g, l, stage)
            if stage == "summ":
                _allgather(g, g.summ_d, g.gath_d, g.b_summd, g.b_gath)
            else:
                ffn_phase(g, l, last, n_exp=n_exp)
                if not last:
                    _halo_exchange(g)
            g.es_stage.close()
    g.s.finish()
    return g


def _maps(inp):
    out = []
    shared = {}
    for l in range(2):
        for k, v in ffn_inputs(inp, l).items():
            shared["%s_L%d" % (k, l)] = v
    zeros_x = None
    for core in range(8):
        m = dict(shared)
        b, sg = core // 4, core % 4
        for l in range(2):
            bi = base_inputs(inp, l, core, inp["x"], inp["ctx"])
            if l == 1:
                del bi["xT"]
            bi.update(mixer_inputs(inp, l, core))
            for k, v in bi.items():
                m["%s_L%d" % (k, l)] = v
        hs = np.zeros((128, 16), np.float32)
        for j in range(8):
            if j // 4 == b and j % 4 == sg - 1:
                hs[:, j] = 1.0
            if j // 4 == b and j % 4 == sg + 1:
                hs[:, 8 + j] = 1.0
        m["hsel"] = hs
        out.append(m)
    return out


def kernel(**inputs):
    inp = {k: np.ascontiguousarray(np.asarray(v), dtype=np.float32) for k, v in inputs.items()}
    g = _build_fused()
    r = run_bass_kernel_spmd(g.nc, _maps(inp), core_ids=list(range(8)))
    out = np.empty_like(inp["x"])
    for core in range(8):
        b, sg = core // 4, core % 4
        out[b, sg * NLAT:(sg + 1) * NLAT] = r.results[core]["outT"].T
    return out
```
